# Optimizing a Trainium2 kernel written in Bass

```python
import jax
import jax.numpy as jnp
from jax import lax
import numpy as np

D_MODEL = 1024
BATCH = 4
SEQ = 4096
DEPTH = 4

N_MIXERS = 3
N_RET = (DEPTH + 2) // 3
N_MLSTM = (DEPTH + 1) // 3
N_RWKV = DEPTH // 3
D_FF = -(-8 * D_MODEL // (3 * 256)) * 256
RMS_EPS = 1e-6

RET_HEADS = 4
RET_QK_DIM = D_MODEL // RET_HEADS
RET_V_DIM = 2 * D_MODEL // RET_HEADS
RET_CHUNK = 128
ROPE_BASE = 10000.0
RET_EPS = 1e-6

MLSTM_INNER = 2 * D_MODEL
MLSTM_HEADS = 4
MLSTM_HEAD_DIM = MLSTM_INNER // MLSTM_HEADS
MLSTM_CONV = 4
MLSTM_QKV_BLOCK = 4
MLSTM_CHUNK = 64
MLSTM_EPS = 1e-6

RWKV_HEAD_DIM = 64
RWKV_HEADS = D_MODEL // RWKV_HEAD_DIM
RWKV_DECAY_LORA = max(32, round(1.8 * D_MODEL ** 0.5 / 32) * 32)
RWKV_ICL_LORA = max(32, round(1.8 * D_MODEL ** 0.5 / 32) * 32)
RWKV_GATE_LORA = max(32, round(0.6 * D_MODEL ** 0.8 / 32) * 32)
RWKV_GN_EPS = 64e-5

kernel_name = 'hybrid_retention_mlstm_rwkv7_trunk'


def _rms_norm(x, g):
    xf = x.astype(jnp.float32)
    y = xf * lax.rsqrt(jnp.mean(xf * xf, axis=-1, keepdims=True) + RMS_EPS)
    return (y * g.astype(jnp.float32)).astype(x.dtype)


def _head_norm(h, eps, center):
    if center:
        h = h - jnp.mean(h, axis=-1, keepdims=True)
    y = h * lax.rsqrt(jnp.mean(h * h, axis=-1, keepdims=True) + eps)
    return y.reshape(h.shape[0], h.shape[1], -1)


def _to_chunks(t, L):
    B, S = t.shape[:2]
    t = t.reshape((B, S // L, L) + t.shape[2:])
    return jnp.swapaxes(jnp.moveaxis(t, 1, 0), 2, 3)


def _from_chunks(t):
    nC, B, H, L, d = t.shape
    return jnp.swapaxes(jnp.moveaxis(t, 0, 1), 2, 3).reshape(B, nC * L, H, d)


def _rotary(t):
    S, d = t.shape[1], t.shape[-1]
    pos = jnp.arange(S, dtype=jnp.float32)
    inv_freq = 1.0 / (ROPE_BASE ** jnp.linspace(0.0, 1.0, d // 2, dtype=jnp.float32))
    ang = pos[:, None] * inv_freq[None, :]
    cos = jnp.cos(ang)[None, :, None, :]
    sin = jnp.sin(ang)[None, :, None, :]
    t1, t2 = t[..., : d // 2], t[..., d // 2:]
    return jnp.concatenate([t1 * cos - t2 * sin, t2 * cos + t1 * sin], axis=-1)


def _retention(x, w_in, gn_g, w_out):
    B, S, _ = x.shape
    H, dk, dv, L = RET_HEADS, RET_QK_DIM, RET_V_DIM, RET_CHUNK
    proj = (x @ w_in).astype(jnp.float32)
    q, k, v, g = jnp.split(proj, [H * dk, 2 * H * dk, 2 * H * dk + H * dv], axis=-1)
    q = _rotary(q.reshape(B, S, H, dk))
    k = _rotary(k.reshape(B, S, H, dk)) * dk ** -0.5
    v = v.reshape(B, S, H, dv)
    log_gamma = jnp.log1p(-2.0 ** (-5.0 - jnp.arange(H, dtype=jnp.float32)))
    idx = jnp.arange(L, dtype=jnp.float32)
    rel = idx[:, None] - idx[None, :]
    intra_decay = jnp.where(rel >= 0, jnp.exp(log_gamma[:, None, None] * jnp.maximum(rel, 0.0)), 0.0)
    q_decay = jnp.exp(log_gamma[:, None] * (idx + 1.0))
    k_decay = jnp.exp(log_gamma[:, None] * (L - 1.0 - idx))
    chunk_decay = jnp.exp(log_gamma * L)

    def step(state, inp):
        qc, kc, vc = inp
        scores = jnp.einsum('bhld,bhmd->bhlm', qc, kc) * intra_decay
        out = jnp.einsum('bhlm,bhmv->bhlv', scores, vc)
        out = out + jnp.einsum('bhld,bhdv->bhlv', qc, state) * q_decay[None, :, :, None]
        state = state * chunk_decay[None, :, None, None] + jnp.einsum(
            'bhld,bhlv->bhdv', kc * k_decay[None, :, :, None], vc)
        return state, out

    state0 = jnp.zeros((B, H, dk, dv), jnp.float32)
    _, o = lax.scan(step, state0, (_to_chunks(q, L), _to_chunks(k, L), _to_chunks(v, L)))
    o = _head_norm(_from_chunks(o), RET_EPS, center=False) * gn_g.astype(jnp.float32)
    y = jax.nn.silu(g) * o
    return y.astype(x.dtype) @ w_out


def _block_diag(t, w):
    B, S, C = t.shape
    nb, bs, _ = w.shape
    return jnp.einsum('bsnc,ncd->bsnd', t.reshape(B, S, nb, bs), w).reshape(B, S, C)


def _causal_conv(u, w, b):
    K, C = w.shape
    y = lax.conv_general_dilated(u, w[:, None, :], window_strides=(1,), padding=[(K - 1, 0)],
                                 dimension_numbers=('NWC', 'WIO', 'NWC'), feature_group_count=C)
    return y + b


def _mlstm(x, w_in, conv_w, conv_b, wq, wk, wv, w_gate, b_gate, gn_g, skip, w_out):
    B, S, _ = x.shape
    H, dh, L = MLSTM_HEADS, MLSTM_HEAD_DIM, MLSTM_CHUNK
    f32 = jnp.float32
    u, z = jnp.split(x @ w_in, 2, axis=-1)
    c = jax.nn.silu(_causal_conv(u, conv_w, conv_b))
    q = _block_diag(c, wq)
    k = _block_diag(c, wk)
    v = _block_diag(u, wv)
    gates = (jnp.concatenate([q, k, v], axis=-1) @ w_gate + b_gate).astype(f32)
    i_pre = gates[..., :H]
    log_f = jax.nn.log_sigmoid(gates[..., H:])
    qh = q.astype(f32).reshape(B, S, H, dh)
    kh = k.astype(f32).reshape(B, S, H, dh) * dh ** -0.5
    vh = v.astype(f32).reshape(B, S, H, dh)
    causal = jnp.tril(jnp.ones((L, L), dtype=bool))

    def step(carry, inp):
        C, n, m = carry
        qc, kc, vc, ic, fc = inp
        b = jnp.cumsum(fc, axis=-1)
        log_d = jnp.where(causal, b[..., :, None] - b[..., None, :] + ic[..., None, :], -jnp.inf)
        log_inter = b + m[..., None]
        m_row = jnp.maximum(log_inter, jnp.max(log_d, axis=-1))
        d_mat = jnp.exp(log_d - m_row[..., None])
        w_inter = jnp.exp(log_inter - m_row)
        s = jnp.einsum('bhld,bhmd->bhlm', qc, kc) * d_mat
        num = jnp.einsum('bhlm,bhmv->bhlv', s, vc) + w_inter[..., None] * jnp.einsum('bhld,bhdv->bhlv', qc, C)
        den = jnp.sum(s, axis=-1) + w_inter * jnp.einsum('bhld,bhd->bhl', qc, n)
        h = num / jnp.maximum(jnp.abs(den), jnp.exp(-m_row))[..., None]
        b_last = b[..., -1]
        log_w = b_last[..., None] - b + ic
        m_new = jnp.maximum(b_last + m, jnp.max(log_w, axis=-1))
        w_k = jnp.exp(log_w - m_new[..., None])
        carry_decay = jnp.exp(b_last + m - m_new)
        C = carry_decay[..., None, None] * C + jnp.einsum('bhld,bhlv->bhdv', kc * w_k[..., None], vc)
        n = carry_decay[..., None] * n + jnp.einsum('bhld,bhl->bhd', kc, w_k)
        return (C, n, m_new), h

    carry0 = (jnp.zeros((B, H, dh, dh), f32), jnp.zeros((B, H, dh), f32), jnp.zeros((B, H), f32))
    _, h = lax.scan(step, carry0, (_to_chunks(qh, L), _to_chunks(kh, L), _to_chunks(vh, L),
                                   _to_chunks(i_pre, L), _to_chunks(log_f, L)))
    h = _head_norm(_from_chunks(h), MLSTM_EPS, center=True) * gn_g
    h = (h + skip * c.astype(f32)) * jax.nn.silu(z.astype(f32))
    return h.astype(x.dtype) @ w_out


def _rwkv7(x, mu, w_rkv, w0, w_lora_a, w_lora_b, a0, a_lora_a, a_lora_b, g_lora_a, g_lora_b,
           k_k, k_a, r_k, gn_g, gn_b, w_out):
    B, S, D = x.shape
    H, dh = RWKV_HEADS, RWKV_HEAD_DIM
    f32 = jnp.float32
    x_prev = jnp.pad(x[:, :-1], ((0, 0), (1, 0), (0, 0)))
    mixes = x[None] + (x_prev - x)[None] * mu[:, None, None, :]
    rkv = jnp.einsum('nbsd,nde->nbse', mixes[:3], w_rkv).astype(f32)
    r, k, v = rkv[0], rkv[1], rkv[2]
    xw, xa, xg = mixes[3], mixes[4], mixes[5]
    w_log = -jax.nn.softplus(-(w0 + jnp.tanh(xw @ w_lora_a) @ w_lora_b).astype(f32)) - 0.5
    decay = jnp.exp(-jnp.exp(w_log))
    a = jax.nn.sigmoid((a0 + (xa @ a_lora_a) @ a_lora_b).astype(f32))
    g = (jax.nn.sigmoid(xg @ g_lora_a) @ g_lora_b).astype(f32)
    kk = (k * k_k).reshape(B, S, H, dh)
    kk = kk * lax.rsqrt(jnp.maximum(jnp.sum(kk * kk, axis=-1, keepdims=True), 1e-24))
    k = k * (1.0 + (a - 1.0) * k_a)

    def heads_time_major(t):
        return jnp.moveaxis(t.reshape(B, S, H, dh), 1, 0)

    def step(state, inp):
        r_t, w_t, k_t, v_t, kk_t, a_t = inp
        sa = jnp.einsum('bhvk,bhk->bhv', state, -kk_t)
        state = (state * w_t[:, :, None, :] + sa[..., None] * (kk_t * a_t)[:, :, None, :]
                 + v_t[..., None] * k_t[:, :, None, :])
        y_t = jnp.einsum('bhvk,bhk->bhv', state, r_t)
        return state, y_t

    state0 = jnp.zeros((B, H, dh, dh), f32)
    _, y = lax.scan(step, state0, (heads_time_major(r), heads_time_major(decay), heads_time_major(k),
                                   heads_time_major(v), jnp.moveaxis(kk, 1, 0), heads_time_major(a)))
    y = jnp.moveaxis(y, 0, 1)
    y = _head_norm(y, RWKV_GN_EPS, center=True) * gn_g + gn_b
    rh, kh, vh = r.reshape(B, S, H, dh), k.reshape(B, S, H, dh), v.reshape(B, S, H, dh)
    bonus = (jnp.sum(rh * kh * r_k.astype(f32), axis=-1, keepdims=True) * vh).reshape(B, S, D)
    out = (y + bonus) * g
    return out.astype(x.dtype) @ w_out


def _swiglu(x, w_gu, w_down):
    gate, up = jnp.split(x @ w_gu, 2, axis=-1)
    return (jax.nn.silu(gate) * up) @ w_down


def setup_inputs(seed: int = 0) -> dict:
    key = jax.random.key(seed)
    ks = iter(jax.random.split(key, 48))
    f32 = jnp.float32

    def nrm(shape, scale):
        return jax.random.normal(next(ks), shape, f32) * scale

    D = D_MODEL
    I = MLSTM_INNER
    HV = RET_HEADS * RET_V_DIM
    ret_cols = 2 * RET_HEADS * RET_QK_DIM + 2 * HV
    nb = I // MLSTM_QKV_BLOCK
    bs = MLSTM_QKV_BLOCK
    x = nrm((BATCH, SEQ, D), 1.0)
    norm_mix = 1.0 + nrm((DEPTH, D), 0.02)
    norm_ffn = 1.0 + nrm((DEPTH, D), 0.02)
    norm_final = 1.0 + nrm((D,), 0.02)
    ret_w_in = nrm((N_RET, D, ret_cols), D ** -0.5)
    ret_gn = 1.0 + nrm((N_RET, HV), 0.02)
    ret_w_out = nrm((N_RET, HV, D), HV ** -0.5)
    ml_w_in = nrm((N_MLSTM, D, 2 * I), D ** -0.5)
    ml_conv_w = nrm((N_MLSTM, MLSTM_CONV, I), MLSTM_CONV ** -0.5)
    ml_conv_b = nrm((N_MLSTM, I), 0.01)
    ml_wq = nrm((N_MLSTM, nb, bs, bs), bs ** -0.5)
    ml_wk = nrm((N_MLSTM, nb, bs, bs), bs ** -0.5)
    ml_wv = nrm((N_MLSTM, nb, bs, bs), bs ** -0.5)
    ml_w_gate = nrm((N_MLSTM, 3 * I, 2 * MLSTM_HEADS), 0.1 * (3 * I) ** -0.5)
    f_bias = jnp.linspace(3.0, 6.0, MLSTM_HEADS, dtype=f32)
    ml_b_gate = jnp.concatenate([nrm((N_MLSTM, MLSTM_HEADS), 0.1),
                                 f_bias[None, :] + nrm((N_MLSTM, MLSTM_HEADS), 0.1)], axis=-1)
    ml_gn = 1.0 + nrm((N_MLSTM, I), 0.02)
    ml_skip = 1.0 + nrm((N_MLSTM, I), 0.02)
    ml_w_out = nrm((N_MLSTM, I, D), I ** -0.5)
    rw_mu = jax.random.uniform(next(ks), (N_RWKV, 6, D), f32)
    rw_w_rkv = nrm((N_RWKV, 3, D, D), D ** -0.5)
    rw_w0 = jnp.linspace(-6.0, -1.0, D, dtype=f32)[None, :] + nrm((N_RWKV, D), 0.1)
    rw_w_lora_a = nrm((N_RWKV, D, RWKV_DECAY_LORA), D ** -0.5)
    rw_w_lora_b = nrm((N_RWKV, RWKV_DECAY_LORA, D), 0.5 * RWKV_DECAY_LORA ** -0.5)
    rw_a0 = nrm((N_RWKV, D), 0.1)
    rw_a_lora_a = nrm((N_RWKV, D, RWKV_ICL_LORA), D ** -0.5)
    rw_a_lora_b = nrm((N_RWKV, RWKV_ICL_LORA, D), 0.5 * RWKV_ICL_LORA ** -0.5)
    rw_g_lora_a = nrm((N_RWKV, D, RWKV_GATE_LORA), D ** -0.5)
    rw_g_lora_b = nrm((N_RWKV, RWKV_GATE_LORA, D), RWKV_GATE_LORA ** -0.5)
    rw_k_k = 0.85 + nrm((N_RWKV, D), 0.05)
    rw_k_a = 1.0 + nrm((N_RWKV, D), 0.05)
    rw_r_k = nrm((N_RWKV, RWKV_HEADS, RWKV_HEAD_DIM), 0.1)
    rw_gn_g = 1.0 + nrm((N_RWKV, D), 0.02)
    rw_gn_b = nrm((N_RWKV, D), 0.01)
    rw_w_out = nrm((N_RWKV, D, D), D ** -0.5)
    ffn_w_gu = nrm((DEPTH, D, 2 * D_FF), D ** -0.5)
    ffn_w_down = nrm((DEPTH, D_FF, D), D_FF ** -0.5)
    return {
        'x': x, 'norm_mix': norm_mix, 'norm_ffn': norm_ffn, 'norm_final': norm_final,
        'ret_w_in': ret_w_in, 'ret_gn': ret_gn, 'ret_w_out': ret_w_out,
        'ml_w_in': ml_w_in, 'ml_conv_w': ml_conv_w, 'ml_conv_b': ml_conv_b,
        'ml_wq': ml_wq, 'ml_wk': ml_wk, 'ml_wv': ml_wv, 'ml_w_gate': ml_w_gate, 'ml_b_gate': ml_b_gate,
        'ml_gn': ml_gn, 'ml_skip': ml_skip, 'ml_w_out': ml_w_out,
        'rw_mu': rw_mu, 'rw_w_rkv': rw_w_rkv, 'rw_w0': rw_w0, 'rw_w_lora_a': rw_w_lora_a,
        'rw_w_lora_b': rw_w_lora_b, 'rw_a0': rw_a0, 'rw_a_lora_a': rw_a_lora_a, 'rw_a_lora_b': rw_a_lora_b,
        'rw_g_lora_a': rw_g_lora_a, 'rw_g_lora_b': rw_g_lora_b, 'rw_k_k': rw_k_k, 'rw_k_a': rw_k_a,
        'rw_r_k': rw_r_k, 'rw_gn_g': rw_gn_g, 'rw_gn_b': rw_gn_b, 'rw_w_out': rw_w_out,
        'ffn_w_gu': ffn_w_gu, 'ffn_w_down': ffn_w_down,
    }


def reference(x, norm_mix, norm_ffn, norm_final, ret_w_in, ret_gn, ret_w_out,
              ml_w_in, ml_conv_w, ml_conv_b, ml_wq, ml_wk, ml_wv, ml_w_gate, ml_b_gate,
              ml_gn, ml_skip, ml_w_out,
              rw_mu, rw_w_rkv, rw_w0, rw_w_lora_a, rw_w_lora_b, rw_a0, rw_a_lora_a, rw_a_lora_b,
              rw_g_lora_a, rw_g_lora_b, rw_k_k, rw_k_a, rw_r_k, rw_gn_g, rw_gn_b, rw_w_out,
              ffn_w_gu, ffn_w_down):
    h = x
    for i in range(DEPTH):
        xn = _rms_norm(h, norm_mix[i])
        kind = i % N_MIXERS
        j = i // N_MIXERS
        if kind == 0:
            y = _retention(xn, ret_w_in[j], ret_gn[j], ret_w_out[j])
        elif kind == 1:
            y = _mlstm(xn, ml_w_in[j], ml_conv_w[j], ml_conv_b[j], ml_wq[j], ml_wk[j], ml_wv[j],
                       ml_w_gate[j], ml_b_gate[j], ml_gn[j], ml_skip[j], ml_w_out[j])
        else:
            y = _rwkv7(xn, rw_mu[j], rw_w_rkv[j], rw_w0[j], rw_w_lora_a[j], rw_w_lora_b[j],
                       rw_a0[j], rw_a_lora_a[j], rw_a_lora_b[j], rw_g_lora_a[j], rw_g_lora_b[j],
                       rw_k_k[j], rw_k_a[j], rw_r_k[j], rw_gn_g[j], rw_gn_b[j], rw_w_out[j])
        h = h + y.astype(h.dtype)
        h = h + _swiglu(_rms_norm(h, norm_ffn[i]), ffn_w_gu[i], ffn_w_down[i]).astype(h.dtype)
    return _rms_norm(h, norm_final)
```

```python
import contextlib
import numpy as np
import ml_dtypes
import concourse.bass as bass
import concourse.mybir as mybir
from concourse.bass_utils import run_bass_kernel_spmd

F32 = mybir.dt.float32
BF16 = mybir.dt.bfloat16
AF = mybir.ActivationFunctionType
ALU = mybir.AluOpType
AX = mybir.AxisListType

SAME_ENGINE_SYNC = True
WAW_RELAX = True
NDMASEM = 8
DEBUG = False
LAST = {}
D = 1024
DFF = 2816
RMS_EPS = 1e-6


class Tile:
    __slots__ = ("t", "lw", "rd", "name")

    def __init__(self, t, name=""):
        self.t = t
        self.lw = None
        self.rd = {}
        self.name = name

    def __getitem__(self, k):
        return self.t[k]


class Eng:
    def __init__(self, fw, name, obj, is_pe=False):
        self.fw = fw
        self.name = name
        self.o = obj
        self.sem = fw.stack.enter_context(fw.nc.semaphore("s_" + name))
        self.n = 0
        self.seen = {}
        self.is_pe = is_pe
        self.dsems = None
        self.dn = 0
        self.last = []

    def wait(self, tok):
        sem, val, en = tok
        k = id(sem)
        if self.seen.get(k, 0) >= val:
            return
        self.seen[k] = val
        self.o.wait_ge(sem, val)


class FW:
    def __init__(self):
        self.nc = bass.Bass("TRN2", target_bir_lowering=False)
        self.stack = contextlib.ExitStack()
        nc = self.nc
        self.pe = Eng(self, "pe", nc.tensor, True)
        self.act = Eng(self, "act", nc.scalar)
        self.dve = Eng(self, "dve", nc.vector)
        self.pool = Eng(self, "pool", nc.gpsimd)
        self.sp = Eng(self, "sp", nc.sync)
        self.engs = [self.pe, self.act, self.dve, self.pool, self.sp]
        for q in (self.sp, self.pool):
            q.dsems = [self.stack.enter_context(nc.semaphore("d_%s%d" % (q.name, i))) for i in range(NDMASEM)]
        self.alltok = []
        self.uid = 0

    def sb(self, shape, dtype, name=None, stack=None):
        self.uid += 1
        name = (name or "t") + "_%d" % self.uid
        t = (stack or self.stack).enter_context(self.nc.sbuf_tensor(name, list(shape), dtype))
        return Tile(t, name)

    def ps(self, shape, dtype, name=None, stack=None):
        self.uid += 1
        name = (name or "p") + "_%d" % self.uid
        t = (stack or self.stack).enter_context(self.nc.psum_tensor(name, list(shape), dtype))
        return Tile(t, name)

    def dram(self, name, shape, dtype, kind=None):
        if kind:
            t = self.nc.dram_tensor(name, list(shape), dtype, kind=kind)
        else:
            t = self.nc.dram_tensor(name, list(shape), dtype)
        return t.ap()

    def _toks(self, r, w):
        toks = []
        for b in r:
            if b.lw is not None:
                toks.append(b.lw)
        for b in w:
            if b.lw is not None:
                toks.append(b.lw)
            toks.extend(b.rd.values())
        return toks

    def _mark(self, tok, r, w, key):
        for b in w:
            b.lw = tok
            b.rd = {}
        for b in r:
            b.rd[key] = tok

    def op(self, eng, fn, r=(), w=()):
        raw = [b.lw for b in r if b.lw is not None]
        for tok in raw:
            if tok[2] == eng.name and (eng.is_pe or not SAME_ENGINE_SYNC):
                continue
            eng.wait(tok)
        for b in w:
            toks = ([b.lw] if b.lw is not None else []) + list(b.rd.values())
            for tok in toks:
                if tok[2] == eng.name and (eng.is_pe or not SAME_ENGINE_SYNC or (WAW_RELAX and eng.name in ("act", "dve"))):
                    continue
                eng.wait(tok)
        ins = fn()
        eng.n += 1
        ins.then_inc(eng.sem, 1)
        tok = (eng.sem, eng.n, eng.name)
        self._mark(tok, r, w, eng.name)
        return tok

    def dma(self, q, out, in_, r=(), w=()):
        i = q.dn % NDMASEM
        use = q.dn // NDMASEM
        sem = q.dsems[i]
        if use > 0:
            q.wait((sem, 16 * use, "dma"))
        for tok in self._toks(r, w):
            q.wait(tok)
        q.o.dma_start(out=out, in_=in_).then_inc(sem, 16)
        q.dn += 1
        tok = (sem, 16 * (use + 1), "dma")
        self._mark(tok, r, w, "dma_%s_%d" % (q.name, i))
        q.last = (q.last + [tok])[-NDMASEM:]
        return tok

    def barrier(self):
        toks = [(e.sem, e.n, e.name) for e in self.engs if e.n > 0]
        toks += self.sp.last + self.pool.last
        for e in self.engs:
            for tok in toks:
                if tok[2] == e.name:
                    continue
                e.wait(tok)

    def finish(self):
        for tok in self.sp.last + self.pool.last:
            self.sp.wait(tok)
        for e in self.engs:
            if e is not self.sp and e.n > 0:
                self.sp.wait((e.sem, e.n, e.name))
        self.barrier()
        nc = self.nc
        sems = [e.sem for e in self.engs]
        for q in (self.sp, self.pool):
            sems += list(q.dsems)
        nc.all_engine_barrier()
        nc.clear_and_free_semaphores(sems)
        nc.all_engine_barrier()


class Prog:
    def __init__(self, S, layers, final_norm=True):
        self.S = S
        self.NT = S // 128
        self.layers = layers
        self.fw = FW()
        fw = self.fw
        nc = fw.nc
        self.nc = nc
        self.ins = {}
        self.ins_ap = {}
        self.x = self.inp("x", [S, D])
        self.out = fw.dram("out", [S, D], F32, "ExternalOutput")
        self.h = fw.dram("h_scr", [S, D], F32)
        self.ht = [Tile(self.h[i * 128:(i + 1) * 128, :], "h%d" % i) for i in range(self.NT)]
        self.xt = [Tile(self.x[i * 128:(i + 1) * 128, :], "x%d" % i) for i in range(self.NT)]
        self.ot = [Tile(self.out[i * 128:(i + 1) * 128, :], "o%d" % i) for i in range(self.NT)]
        self.ident = fw.sb([128, 128], BF16, "ident")
        idd = self.inp("c_ident", [128, 128])
        fw.dma(fw.pool, self.ident[:], idd[:, :], w=[self.ident])
        self.P = [fw.ps([128, 512], F32, "bank%d" % i) for i in range(8)]
        for e in (1e-6, 64e-5):
            self.eps_tile(e)
        self.first = True
        for (kind, li, j) in layers:
            if kind == "ffn":
                self.ffn(li)
            elif kind == "ret":
                self.retention(li, j)
            elif kind == "mlstm":
                self.mlstm(li, j)
            elif kind == "rwkv":
                self.rwkv(li, j)
            fw.barrier()
        if final_norm:
            self.final_norm()
        else:
            self.copy_out()
        fw.finish()

    def inp(self, name, shape, dtype=F32):
        if name in self.ins:
            return self.ins_ap[name]
        ap = self.fw.dram(name, shape, dtype, "ExternalInput")
        self.ins[name] = (tuple(shape), dtype)
        self.ins_ap[name] = ap
        return ap

    def src_tiles(self):
        if self.first:
            return self.xt
        return self.ht

    def mm(self, out, lhsT, rhs, start, stop, r, w, sgc=False):
        nc = self.nc
        if sgc:
            return self.fw.op(self.fw.pe, lambda: nc.tensor.matmul(out, lhsT, rhs, start=start, stop=stop, skip_group_check=True), r, w)
        return self.fw.op(self.fw.pe, lambda: nc.tensor.matmul(out, lhsT, rhs, start=start, stop=stop), r, w)

    def outproj(self, y_, yT_, wout, hbuf, c, nk=16):
        fw = self.fw
        P = self.P
        self.transpose_to(y_, yT_, 0, nk, [P[6], P[7]], [fw.act, fw.dve])
        for half in range(2):
            pb = P[4 + half]
            for k in range(nk):
                self.mm(pb[:, :], yT_[:, k, :], wout[:, k, half * 512:(half + 1) * 512], k == 0, k == nk - 1, [yT_, wout], [pb])
            self.tt(fw.dve, hbuf[:, half * 512:(half + 1) * 512], hbuf[:, half * 512:(half + 1) * 512], pb[:, :], ALU.add,
                    [hbuf, pb], [hbuf])
        fw.dma(fw.sp, self.ht[c].t, hbuf[:], r=[hbuf], w=[self.ht[c]])

    def actf(self, out, in_, func, r, w, scale=1.0, bias=0.0, accum_out=None):
        nc = self.nc
        if accum_out is not None:
            return self.fw.op(self.fw.act, lambda: nc.scalar.activation(out=out, in_=in_, func=func, scale=scale, bias=bias, accum_out=accum_out), r, w)
        return self.fw.op(self.fw.act, lambda: nc.scalar.activation(out=out, in_=in_, func=func, scale=scale, bias=bias), r, w)

    def tt(self, eng, out, in0, in1, op, r, w):
        return self.fw.op(eng, lambda: eng.o.tensor_tensor(out=out, in0=in0, in1=in1, op=op), r, w)

    def ts(self, eng, out, in0, s1, s2, op0, op1, r, w):
        if s2 is None:
            return self.fw.op(eng, lambda: eng.o.tensor_scalar(out=out, in0=in0, scalar1=s1, scalar2=None, op0=op0), r, w)
        return self.fw.op(eng, lambda: eng.o.tensor_scalar(out=out, in0=in0, scalar1=s1, scalar2=s2, op0=op0, op1=op1), r, w)

    def stt(self, eng, out, in0, scalar, in1, op0, op1, r, w):
        if eng is self.fw.pool:
            eng = self.fw.dve
        return self.fw.op(eng, lambda: eng.o.scalar_tensor_tensor(out=out, in0=in0, scalar=scalar, in1=in1, op0=op0, op1=op1), r, w)

    def split2(self, src_ap, src_tiles, hi, lo, tmp, hi_ap=None, lo_ap=None, tmp_ap=None):
        fw = self.fw
        nc = self.nc
        hi_ap = hi[:] if hi_ap is None else hi_ap
        lo_ap = lo[:] if lo_ap is None else lo_ap
        tmp_ap = tmp[:] if tmp_ap is None else tmp_ap
        fw.op(fw.pool, lambda: nc.gpsimd.tensor_copy(out=hi_ap, in_=src_ap), list(src_tiles), [hi])
        self.tt(fw.dve, tmp_ap, src_ap, hi_ap, ALU.subtract, list(src_tiles) + [hi], [tmp])
        fw.op(fw.pool, lambda: nc.gpsimd.tensor_copy(out=lo_ap, in_=tmp_ap), [tmp], [lo])

    def rstd_from_ss(self, ss, std, rstd, n, eps):
        fw = self.fw
        self.actf(std[:], ss[:], AF.Sqrt, [ss], [std], scale=1.0 / n, bias=self.eps_tile(eps))
        fw.op(fw.dve, lambda: self.nc.vector.reciprocal(out=rstd[:], in_=std[:]), [std], [rstd])

    def eps_tile(self, eps):
        key = "eps_%g" % eps
        if not hasattr(self, "_eps"):
            self._eps = {}
        if key not in self._eps:
            t = self.fw.sb([128, 1], F32, "eps")
            self.fw.op(self.fw.pool, lambda: self.nc.gpsimd.memset(t[:], eps), [], [t])
            self._eps[key] = t
        return self._eps[key][:]

    def norm_tile(self, src, gbc, hbuf, xn, st, sq):
        fw = self.fw
        nc = self.nc
        ss, std, rstd = st
        fw.dma(fw.sp, hbuf[:], src.t, r=[src], w=[hbuf])
        self.actf(sq[:], hbuf[:], AF.Square, [hbuf], [sq, ss], accum_out=ss[:])
        self.rstd_from_ss(ss, std, rstd, D, RMS_EPS)
        self.stt(fw.dve, xn[:], hbuf[:], rstd[:], gbc[:], ALU.mult, ALU.mult, [hbuf, rstd, gbc], [xn])

    def transpose_to(self, xn, dstT, col0, nk, pbanks, evac_engs):
        fw = self.fw
        for g in range(0, nk, 4):
            pb = pbanks[(g // 4) % len(pbanks)]
            n = min(4, nk - g)
            for k in range(n):
                self.mm(pb[:, k * 128:(k + 1) * 128], xn[:, (g + k) * 128:(g + k + 1) * 128], self.ident[:], True, True,
                        [xn, self.ident], [pb])
            e = evac_engs[(g // 4) % len(evac_engs)]
            src = pb[:, 0:n * 128].rearrange("p (k t) -> p k t", k=n)
            dst = dstT[:, g:g + n, col0:col0 + 128]
            if e is fw.act:
                self.actf(dst, src, AF.Copy, [pb], [dstT])
            else:
                fw.op(e, lambda: e.o.tensor_copy(out=dst, in_=src), [pb], [dstT])

    def load_w_bf16(self, dst, src_ap, K, N, r=(), col0=0, ncols=None):
        fw = self.fw
        ncols = ncols or N
        kc = K // 128
        for k in range(kc):
            fw.dma(fw.pool, dst[:, k, 0:ncols], src_ap[k * 128:(k + 1) * 128, col0:col0 + ncols], w=[dst])


    def norm_all(self, gname, xnT_all, st):
        fw = self.fw
        P = self.P
        g_d = self.inp(gname, [128, D])
        gbc = fw.sb([128, D], F32, "gbc", st)
        hb = [fw.sb([128, D], F32, "hb%d" % i, st) for i in range(2)]
        xn = [fw.sb([128, D], BF16, "xn%d" % i, st) for i in range(2)]
        sq = fw.sb([128, D], BF16, "sq", st)
        stt_ = [[fw.sb([128, 1], F32, "st", st) for _ in range(3)] for _ in range(2)]
        fw.dma(fw.sp, gbc[:], g_d[:, :], w=[gbc])
        src = self.src_tiles()
        for ti in range(self.NT):
            self.norm_tile(src[ti], gbc, hb[ti % 2], xn[ti % 2], stt_[ti % 2], sq)
            self.transpose_to(xn[ti % 2], xnT_all, ti * 128, 8, [P[6], P[7]], [fw.act, fw.dve])

    def stream_fm(self, w_d, col0, xnT_all, wblks, bi, cb_fn):
        fw = self.fw
        S = self.S
        P = self.P
        wb = wblks[bi % 2]
        self.load_w_bf16(wb, w_d, D, None, col0=col0, ncols=512)
        TT = min(512, S)
        n = 0
        for tt in range(S // TT):
            for c in range(4):
                pb = P[(4 * tt + c) % 4]
                for k in range(8):
                    self.mm(pb[:, 0:TT], wb[:, k, c * 128:(c + 1) * 128], xnT_all[:, k, tt * TT:(tt + 1) * TT], k == 0, k == 7,
                            [wb, xnT_all], [pb])
                cb_fn(c, tt, pb, TT)

    def stream_tm(self, w_d, col0, xnT_all, wblks, bi, cb_fn, K=D, ncols=512):
        fw = self.fw
        P = self.P
        wb = wblks[bi % 2]
        self.load_w_bf16(wb, w_d, K, None, col0=col0, ncols=ncols)
        kc = K // 128
        for t in range(self.NT):
            pb = P[4 + (t % 2)]
            for k in range(kc):
                self.mm(pb[:, 0:ncols], xnT_all[:, k, t * 128:(t + 1) * 128], wb[:, k, 0:ncols], k == 0, k == kc - 1,
                        [wb, xnT_all], [pb])
            cb_fn(t, pb)

    def retention(self, li, j):
        fw = self.fw
        nc = self.nc
        S = self.S
        NT = self.NT
        P = self.P
        H, DK, DV, L = 4, 256, 512, 128
        w_in = self.inp("ret_w_in_%d" % j, [D, 6144])
        w_out = self.inp("ret_w_out_%d" % j, [2048, D])
        gn_d = self.inp("ret_gn_bc_%d" % j, [128, 2048])
        cos_d = self.inp("c_cosT", [128, S])
        sin_d = self.inp("c_sinT", [128, S])
        mask_d = self.inp("c_ret_maskT", [128, H * 128])
        dec_d = self.inp("c_ret_dec", [128, 8])
        if not hasattr(self, "ret_scr"):
            kd_ = "ExternalOutput" if DEBUG else None
            self.ret_scr = (fw.dram("ret_qkT", [16, 128, S], BF16, kd_), fw.dram("ret_v", [S, 2048], BF16, kd_),
                            fw.dram("ret_gs", [S, 2048], BF16, kd_))
        qk_d, v_d, gs_d = self.ret_scr
        gam = [1.0 - 2.0 ** (-5.0 - h) for h in range(H)]
        cd = [float(np.float32(g) ** L) for g in gam]
        TT = min(512, S)
        with contextlib.ExitStack() as st:
            xnT_all = fw.sb([128, 8, S], BF16, "xnT_all", st)
            cosT = fw.sb([128, S], F32, "cosT", st)
            sinT = fw.sb([128, S], F32, "sinT", st)
            fw.dma(fw.sp, cosT[:], cos_d[:, :], w=[cosT])
            fw.dma(fw.sp, sinT[:], sin_d[:, :], w=[sinT])
            wblks = [fw.sb([128, 8, 512], BF16, "wblk%d" % i, st) for i in range(2)]
            c0 = [fw.sb([128, 512], F32, "c0_%d" % i, st) for i in range(2)]
            c1 = [fw.sb([128, 512], F32, "c1_%d" % i, st) for i in range(2)]
            t1 = fw.sb([128, 512], F32, "t1", st)
            t2 = fw.sb([128, 512], F32, "t2", st)
            t3 = fw.sb([128, 512], F32, "t3", st)
            t4 = fw.sb([128, 512], F32, "t4", st)
            o1 = [fw.sb([128, 512], BF16, "o1_%d" % i, st) for i in range(2)]
            o2 = [fw.sb([128, 512], BF16, "o2_%d" % i, st) for i in range(2)]
            vst = [fw.sb([128, 512], BF16, "vst%d" % i, st) for i in range(4)]
            with contextlib.ExitStack() as st2:
                self.norm_all("norm_mix_bc_%d" % li, xnT_all, st2)
            cnt = [0]
            hold = {}
            for blk in range(4):
                sc = 1.0 if blk < 2 else DK ** -0.5

                def cb(c, tt, pb, TT_, blk=blk, sc=sc):
                    half = c % 2
                    i = cnt[0] % 2
                    dst = c0[i] if half == 0 else c1[i]
                    self.actf(dst[:, 0:TT_], pb[:, 0:TT_], AF.Copy, [pb], [dst], scale=sc)
                    if half == 0:
                        return
                    a, b = c0[i], c1[i]
                    cs = cosT[:, tt * TT_:(tt + 1) * TT_]
                    sn = sinT[:, tt * TT_:(tt + 1) * TT_]
                    self.tt(fw.dve, t1[:, 0:TT_], a[:, 0:TT_], cs, ALU.mult, [a, cosT], [t1])
                    self.tt(fw.pool, t2[:, 0:TT_], b[:, 0:TT_], sn, ALU.mult, [b, sinT], [t2])
                    self.tt(fw.pool, t3[:, 0:TT_], b[:, 0:TT_], cs, ALU.mult, [b, cosT], [t3])
                    self.tt(fw.dve, t4[:, 0:TT_], a[:, 0:TT_], sn, ALU.mult, [a, sinT], [t4])
                    self.tt(fw.dve, o1[i][:, 0:TT_], t1[:, 0:TT_], t2[:, 0:TT_], ALU.subtract, [t1, t2], [o1[i]])
                    self.tt(fw.pool, o2[i][:, 0:TT_], t3[:, 0:TT_], t4[:, 0:TT_], ALU.add, [t3, t4], [o2[i]])
                    cc = blk * 4 + c - 1
                    fw.dma(fw.sp, qk_d[cc, :, tt * TT_:(tt + 1) * TT_], o1[i][:, 0:TT_], r=[o1[i]])
                    fw.dma(fw.sp, qk_d[cc + 1, :, tt * TT_:(tt + 1) * TT_], o2[i][:, 0:TT_], r=[o2[i]])
                    cnt[0] += 1

                self.stream_fm(w_in, blk * 512, xnT_all, wblks, blk, cb)
            for blk in range(8):
                def cb(t, pb, blk=blk):
                    i = cnt[0] % 4
                    cnt[0] += 1
                    if blk < 4:
                        if t % 2 == 0:
                            self.actf(vst[i][:], pb[:, :], AF.Copy, [pb], [vst[i]])
                        else:
                            fw.op(fw.dve, lambda: nc.vector.tensor_copy(out=vst[i][:], in_=pb[:, :]), [pb], [vst[i]])
                        fw.dma(fw.sp, v_d[t * 128:(t + 1) * 128, blk * 512:(blk + 1) * 512], vst[i][:], r=[vst[i]])
                    else:
                        self.actf(vst[i][:], pb[:, :], AF.Silu, [pb], [vst[i]])
                        fw.dma(fw.sp, gs_d[t * 128:(t + 1) * 128, (blk - 4) * 512:(blk - 3) * 512], vst[i][:], r=[vst[i]])

                self.stream_tm(w_in, 2048 + blk * 512, xnT_all, wblks, blk, cb)
            fw.barrier()
        with contextlib.ExitStack() as st:
            wout = fw.sb([128, 16, D], BF16, "wout", st)
            self.load_w_bf16(wout, w_out, 2048, D)
            gnbc = fw.sb([128, 2048], F32, "gnbc", st)
            maskT = fw.sb([128, H * 128], F32, "maskT", st)
            dec = fw.sb([128, 8], F32, "dec", st)
            fw.dma(fw.sp, gnbc[:], gn_d[:, :], w=[gnbc])
            fw.dma(fw.sp, maskT[:], mask_d[:, :], w=[maskT])
            fw.dma(fw.sp, dec[:], dec_d[:, :], w=[dec])
            qk = [fw.sb([128, 16, TT], BF16, "qk%d" % i, st) for i in range(2)]
            vt = [fw.sb([128, 2048], BF16, "vt%d" % i, st) for i in range(2)]
            gt = [fw.sb([128, 2048], BF16, "gt%d" % i, st) for i in range(2)]
            ggn = [fw.sb([128, 2048], F32, "ggn%d" % i, st) for i in range(2)]
            Sst = [fw.sb([128, 2, DV], F32, "S%d" % h, st) for h in range(H)]
            Sbf = [fw.sb([128, 2, DV], BF16, "Sbf%d" % h, st) for h in range(H)]
            kd = [fw.sb([128, DK], BF16, "kd%d" % i, st) for i in range(2)]
            sT = [fw.sb([128, 128], BF16, "sT%d" % i, st) for i in range(2)]
            tmp = [fw.sb([128, DV], F32, "tmp%d" % i, st) for i in range(2)]
            ob = [fw.sb([128, DV], F32, "ob%d" % i, st) for i in range(2)]
            sq = fw.sb([128, DV], BF16, "sq", st)
            stt_ = [[fw.sb([128, 1], F32, "st", st) for _ in range(3)] for _ in range(2)]
            yb = [fw.sb([128, 2048], BF16, "yb%d" % i, st) for i in range(2)]
            yT = [fw.sb([128, 16, 128], BF16, "yT%d" % i, st) for i in range(2)]
            hb = [fw.sb([128, D], F32, "hb%d" % i, st) for i in range(2)]
            for h in range(H):
                fw.op(fw.pool, lambda: nc.gpsimd.memset(Sst[h][:], 0.0), [], [Sst[h]])
                fw.op(fw.pool, lambda: nc.gpsimd.memset(Sbf[h][:], 0.0), [], [Sbf[h]])
            src = self.src_tiles()
            cpt = TT // 128
            n = 0
            for c in range(NT):
                tt, cl = c // cpt, c % cpt
                q_ = qk[tt % 2]
                if cl == 0:
                    fw.dma(fw.sp, q_[:], qk_d[:, :, tt * TT:(tt + 1) * TT].rearrange("c p t -> p c t"), w=[q_])
                v_, g_, gg = vt[c % 2], gt[c % 2], ggn[c % 2]
                fw.dma(fw.sp, v_[:], v_d[c * 128:(c + 1) * 128, :], w=[v_])
                fw.dma(fw.sp, g_[:], gs_d[c * 128:(c + 1) * 128, :], w=[g_])
                fw.dma(fw.sp, hb[c % 2][:], src[c].t, r=[src[c]], w=[hb[c % 2]])
                self.tt(fw.pool, gg[:], g_[:], gnbc[:], ALU.mult, [g_, gnbc], [gg])
                y_ = yb[c % 2]
                cs = slice(cl * 128, (cl + 1) * 128)
                for h in range(H):
                    i = n % 2
                    n += 1
                    for half in range(2):
                        self.mm(P[0][:, half * 128:(half + 1) * 128], q_[:, 8 + 2 * h + half, cs], self.ident[:], True, True,
                                [q_, self.ident], [P[0]])
                    self.actf(kd[i][:], P[0][:, 0:DK], AF.Copy, [P[0], dec], [kd[i]], scale=dec[:, h:h + 1])
                    for half in range(2):
                        self.mm(P[1][:, 0:128], q_[:, 8 + 2 * h + half, cs], q_[:, 2 * h + half, cs], half == 0, half == 1,
                                [q_], [P[1]])
                    self.tt(fw.dve, sT[i][:], P[1][:, 0:128], maskT[:, h * 128:(h + 1) * 128], ALU.mult, [P[1], maskT], [sT[i]])
                    self.mm(P[2][:, :], sT[i][:], v_[:, h * DV:(h + 1) * DV], True, True, [sT[i], v_], [P[2]])
                    for half in range(2):
                        self.mm(P[3][:, :], q_[:, 2 * h + half, cs], Sbf[h][:, half, :], half == 0, half == 1, [q_, Sbf[h]], [P[3]])
                    self.actf(tmp[i][:], P[3][:, :], AF.Copy, [P[3], dec], [tmp[i]], scale=dec[:, 4 + h:5 + h])
                    self.tt(fw.dve, ob[i][:], tmp[i][:], P[2][:, :], ALU.add, [tmp[i], P[2]], [ob[i]])
                    ss, std, rstd = stt_[i]
                    self.actf(sq[:], ob[i][:], AF.Square, [ob[i]], [sq, ss], accum_out=ss[:])
                    self.rstd_from_ss(ss, std, rstd, DV, 1e-6)
                    self.stt(fw.dve, y_[:, h * DV:(h + 1) * DV], ob[i][:], rstd[:], gg[:, h * DV:(h + 1) * DV], ALU.mult, ALU.mult,
                             [ob[i], rstd, gg], [y_])
                    for half in range(2):
                        self.mm(P[4 + half][:, :], kd[i][:, half * 128:(half + 1) * 128], v_[:, h * DV:(h + 1) * DV], True, True,
                                [kd[i], v_], [P[4 + half]])
                    for half in range(2):
                        self.stt(fw.dve, Sst[h][:, half, :], Sst[h][:, half, :], cd[h], P[4 + half][:, :], ALU.mult, ALU.add,
                                 [P[4 + half], Sst[h]], [Sst[h]])
                    fw.op(fw.pool, lambda: nc.gpsimd.tensor_copy(out=Sbf[h][:], in_=Sst[h][:]), [Sst[h]], [Sbf[h]])
                yT_ = yT[c % 2]
                self.transpose_to(y_, yT_, 0, 16, [P[6], P[7]], [fw.act, fw.dve])
                for half in range(2):
                    pb = P[4 + half]
                    for k in range(16):
                        self.mm(pb[:, :], yT_[:, k, :], wout[:, k, half * 512:(half + 1) * 512], k == 0, k == 15, [yT_, wout], [pb])
                    self.tt(fw.dve, hb[c % 2][:, half * 512:(half + 1) * 512], hb[c % 2][:, half * 512:(half + 1) * 512], pb[:, :], ALU.add,
                            [hb[c % 2], pb], [hb[c % 2]])
                fw.dma(fw.sp, self.ht[c].t, hb[c % 2][:], r=[hb[c % 2]], w=[self.ht[c]])
            fw.barrier()
        self.first = False


    def mlstm(self, li, j):
        fw = self.fw
        nc = self.nc
        S = self.S
        NT = self.NT
        P = self.P
        H, DH = 4, 512
        NC4 = NT * 4
        w_in = self.inp("ml_w_in_%d" % j, [D, 4096])
        w_out = self.inp("ml_w_out_%d" % j, [2048, D])
        bd_d = self.inp("ml_bd_%d" % j, [128, 48 * 128])
        wg_d = self.inp("ml_wg_%d" % j, [128, 48 * 8])
        cw_d = self.inp("ml_cw_%d" % j, [128, 16 * 4])
        cb_d = self.inp("ml_cb_%d" % j, [128, 16])
        bg_d = self.inp("ml_bg_bc_%d" % j, [128, NT * 8])
        gn_d = self.inp("ml_gn_bc_%d" % j, [128, 2048])
        sk_d = self.inp("ml_skip_bc_%d" % j, [128, 2048])
        tri_d = self.inp("c_tri", [128, 128])
        idf_d = self.inp("c_identf", [128, 128])
        if not hasattr(self, "ml_scr"):
            self.ml_scr = (fw.dram("ml_qkT", [32, 128, S], BF16), fw.dram("ml_v", [S, 2048], BF16),
                           fw.dram("ml_c", [S, 2048], BF16), fw.dram("ml_zs", [S, 2048], BF16))
        qk_d, v_d, c_d, zs_d = self.ml_scr
        TT = min(512, S)
        NTT = S // TT
        with contextlib.ExitStack() as ph:
            KS = fw.sb([128, NC4], F32, "KS", ph)
            RS = fw.sb([128, NC4], F32, "RS", ph)
            SC = fw.sb([128, NC4], F32, "SC", ph)
            tri = fw.sb([128, 128], F32, "tri", ph)
            idf = fw.sb([128, 128], F32, "idf", ph)
            onesf = fw.sb([128, 128], F32, "onesf", ph)
            onesb = fw.sb([128, 1], BF16, "onesb", ph)
            one1 = fw.sb([128, 1], F32, "one1", ph)
            fw.dma(fw.sp, tri[:], tri_d[:, :], w=[tri])
            fw.dma(fw.sp, idf[:], idf_d[:, :], w=[idf])
            fw.op(fw.pool, lambda: nc.gpsimd.memset(onesf[:], 1.0), [], [onesf])
            fw.op(fw.pool, lambda: nc.gpsimd.memset(onesb[:], 1.0), [], [onesb])
            fw.op(fw.pool, lambda: nc.gpsimd.memset(one1[:], 1.0), [], [one1])
            with contextlib.ExitStack() as st:
                xnT_all = fw.sb([128, 8, S], BF16, "xnT_all", st)
                wblk = [fw.sb([128, 8, 128], BF16, "wblk%d" % i, st) for i in range(2)]
                wblk2 = [fw.sb([128, 8, 512], BF16, "wblkz%d" % i, st) for i in range(2)]
                bd = fw.sb([128, 48, 128], BF16, "bd", st)
                wg = fw.sb([128, 48, 8], BF16, "wg", st)
                cw = fw.sb([128, 16, 4], F32, "cw", st)
                cb = fw.sb([128, 16], F32, "cb", st)
                bg = fw.sb([128, NT * 8], F32, "bg", st)
                uT = fw.sb([128, S + 3], F32, "uT", st)
                acc = fw.sb([128, S], F32, "acc", st)
                cTb = fw.sb([128, S], BF16, "cTb", st)
                uTb = fw.sb([128, S], BF16, "uTb", st)
                stg = [fw.sb([128, TT], BF16, "stg%d" % i, st) for i in range(3)]
                stgv = [fw.sb([128, 4, 128], BF16, "stgv%d" % i, st) for i in range(2)]
                stgc = [fw.sb([128, 4, 128], BF16, "stgc%d" % i, st) for i in range(2)]
                vst = [fw.sb([128, 512], BF16, "vst%d" % i, st) for i in range(2)]
                G = fw.sb([128, NT, 8], F32, "G", st)
                E = fw.sb([128, NT, 4], F32, "E", st)
                SPt = fw.sb([128, NT, 4], F32, "SPt", st)
                A = fw.sb([128, NC4], F32, "A", st)
                Bn = fw.sb([128, NC4], F32, "Bn", st)
                tmpk = fw.sb([128, NC4], F32, "tmpk", st)
                mloc = fw.sb([128, 1], F32, "mloc", st)
                rows = fw.sb([1, 6, 128], F32, "rows", st)
                sph = fw.sb([128, NC4], BF16, "sph", st)
                spl = fw.sb([128, NC4], BF16, "spl", st)
                mlh = fw.sb([128, 1], BF16, "mlh", st)
                mll = fw.sb([128, 1], BF16, "mll", st)
                mlt = fw.sb([128, 1], F32, "mlt", st)
                rwh = fw.sb([1, 2, 128], BF16, "rwh", st)
                rwl = fw.sb([1, 2, 128], BF16, "rwl", st)
                rwt = fw.sb([1, 2, 128], F32, "rwt", st)
                trib = fw.sb([128, 128], BF16, "trib", st)
                onesb2 = fw.sb([128, 128], BF16, "onesb2", st)
                fw.op(fw.pool, lambda: nc.gpsimd.tensor_copy(out=trib[:], in_=tri[:]), [tri], [trib])
                fw.op(fw.pool, lambda: nc.gpsimd.memset(onesb2[:], 1.0), [], [onesb2])
                fw.dma(fw.pool, bd[:].rearrange("p a b -> p (a b)"), bd_d[:, :], w=[bd])
                fw.dma(fw.pool, wg[:].rearrange("p a b -> p (a b)"), wg_d[:, :], w=[wg])
                fw.dma(fw.sp, cw[:].rearrange("p a b -> p (a b)"), cw_d[:, :], w=[cw])
                fw.dma(fw.sp, cb[:], cb_d[:, :], w=[cb])
                fw.dma(fw.sp, bg[:], bg_d[:, :], w=[bg])
                with contextlib.ExitStack() as st2:
                    self.norm_all("norm_mix_bc_%d" % li, xnT_all, st2)
                    fw.barrier()
                fw.op(fw.dve, lambda: nc.vector.memset(P[7][:, :], 0.0), [], [P[7]])
                fw.op(fw.pool, lambda: nc.gpsimd.memset(uT[:, 0:3], 0.0), [], [uT])
                n = 0
                for cc in range(16):
                    wb = wblk[cc % 2]
                    self.load_w_bf16(wb, w_in, D, None, col0=cc * 128, ncols=128)
                    for tt in range(NTT):
                        pb = P[tt % 4]
                        for k in range(8):
                            self.mm(pb[:, 0:TT], wb[:, k, :], xnT_all[:, k, tt * TT:(tt + 1) * TT], k == 0, k == 7, [wb, xnT_all], [pb])
                        self.actf(uT[:, 3 + tt * TT:3 + (tt + 1) * TT], pb[:, 0:TT], AF.Copy, [pb], [uT])
                    e1 = fw.dve if cc % 2 == 0 else fw.pool
                    self.ts(e1, acc[:], uT[:, 0:S], cw[:, cc, 0:1], cb[:, cc:cc + 1], ALU.mult, ALU.add, [uT, cw, cb], [acc])
                    for jj in range(1, 4):
                        self.stt(e1, acc[:], uT[:, jj:jj + S], cw[:, cc, jj:jj + 1], acc[:], ALU.mult, ALU.add, [uT, cw, acc], [acc])
                    self.actf(cTb[:], acc[:], AF.Silu, [acc], [cTb])
                    e2 = fw.pool if cc % 2 == 0 else fw.dve
                    fw.op(e2, lambda: e2.o.tensor_copy(out=uTb[:], in_=uT[:, 3:3 + S]), [uT], [uTb])
                    for tt in range(NTT):
                        ts_ = slice(tt * TT, (tt + 1) * TT)
                        for wi, (srcT, gi) in enumerate(((cTb, cc), (cTb, 16 + cc), (uTb, 32 + cc))):
                            pb = P[4 + (n % 2)]
                            sg_ = stg[n % 3]
                            n += 1
                            self.mm(pb[:, 0:TT], bd[:, gi, :], srcT[:, ts_], True, True, [bd, srcT], [pb])
                            if wi == 1:
                                self.actf(sg_[:, 0:TT], pb[:, 0:TT], AF.Copy, [pb], [sg_])
                            else:
                                fw.op(fw.dve, lambda: nc.vector.tensor_copy(out=sg_[:, 0:TT], in_=pb[:, 0:TT]), [pb], [sg_])
                            if wi < 2:
                                fw.dma(fw.sp, qk_d[wi * 16 + cc, :, ts_], sg_[:, 0:TT], r=[sg_])
                            for t4 in range(TT // 128):
                                t = tt * (TT // 128) + t4
                                self.mm(P[7][:, t * 8:(t + 1) * 8], sg_[:, t4 * 128:(t4 + 1) * 128], wg[:, gi, :], False, False,
                                        [sg_, wg], [P[7]], sgc=True)
                        nq = TT // 128
                        sv, sc_ = stgv[tt % 2], stgc[tt % 2]
                        for t4 in range(nq):
                            self.mm(P[6][:, t4 * 128:(t4 + 1) * 128], uTb[:, tt * TT + t4 * 128:tt * TT + (t4 + 1) * 128], bd[:, 32 + cc, :],
                                    True, True, [uTb, bd], [P[6]])
                        self.actf(sv[:, 0:nq, :], P[6][:, 0:nq * 128].rearrange("p (a b) -> p a b", a=nq), AF.Copy, [P[6]], [sv])
                        fw.dma(fw.sp, v_d[tt * TT:(tt + 1) * TT, cc * 128:(cc + 1) * 128].rearrange("(a p) c -> p a c", p=128),
                               sv[:, 0:nq, :], r=[sv])
                        for t4 in range(nq):
                            self.mm(P[6][:, t4 * 128:(t4 + 1) * 128], cTb[:, tt * TT + t4 * 128:tt * TT + (t4 + 1) * 128], self.ident[:],
                                    True, True, [cTb, self.ident], [P[6]])
                        fw.op(fw.dve, lambda: nc.vector.tensor_copy(out=sc_[:, 0:nq, :], in_=P[6][:, 0:nq * 128].rearrange("p (a b) -> p a b", a=nq)),
                              [P[6]], [sc_])
                        fw.dma(fw.sp, c_d[tt * TT:(tt + 1) * TT, cc * 128:(cc + 1) * 128].rearrange("(a p) c -> p a c", p=128),
                               sc_[:, 0:nq, :], r=[sc_])
                cnt = [0]
                for blk in range(4):
                    def cbz(t, pb, blk=blk):
                        i = cnt[0] % 2
                        cnt[0] += 1
                        self.actf(vst[i][:], pb[:, :], AF.Silu, [pb], [vst[i]])
                        fw.dma(fw.sp, zs_d[t * 128:(t + 1) * 128, blk * 512:(blk + 1) * 512], vst[i][:], r=[vst[i]])
                    self.stream_tm(w_in, 2048 + blk * 512, xnT_all, wblk2, blk, cbz)
                G2 = G[:].rearrange("p a b -> p (a b)")
                self.tt(fw.dve, G2, P[7][:, 0:NT * 8], bg[:], ALU.add, [P[7], bg], [G])
                self.actf(E[:], G[:, :, 4:8], AF.Exp, [G], [E], scale=-1.0)
                self.actf(SPt[:], E[:], AF.Ln, [E, one1], [SPt], bias=one1[:])
                SP2 = SPt[:].rearrange("p a b -> p (a b)")
                self.split2(SP2, [SPt], sph, spl, tmpk)
                for pi, part in enumerate((sph, spl)):
                    self.mm(P[0][:, 0:NC4], trib[:], part[:], pi == 0, pi == 1, [trib, part], [P[0]])
                for pi, part in enumerate((sph, spl)):
                    self.mm(P[1][:, 0:NC4], onesb2[:], part[:], pi == 0, pi == 1, [onesb2, part], [P[1]])
                self.tt(fw.dve, A[:].rearrange("p (a b) -> p a b", b=4), G[:, :, 0:4], P[0][:, 0:NC4].rearrange("p (a b) -> p a b", b=4),
                        ALU.add, [G, P[0]], [A])
                fw.op(fw.dve, lambda: nc.vector.tensor_copy(out=Bn[:], in_=P[0][:, 0:NC4]), [P[0]], [Bn])
                self.split2(A[:], [A], sph, spl, tmpk)
                for pi, part in enumerate((sph, spl)):
                    self.mm(P[2][0:NC4, 0:128], part[:], self.ident[:], pi == 0, pi == 1, [part, self.ident], [P[2]])
                fw.op(fw.dve, lambda: nc.vector.reduce_max(out=mloc[0:NC4, :], in_=P[2][0:NC4, 0:128], axis=AX.X), [P[2]], [mloc])
                self.split2(mloc[0:NC4, :], [mloc], mlh, mll, mlt, hi_ap=mlh[0:NC4, :], lo_ap=mll[0:NC4, :], tmp_ap=mlt[0:NC4, :])
                for pi, part in enumerate((mlh, mll)):
                    self.mm(P[3][0:1, 0:NC4], part[0:NC4, 0:1], self.ident[0:NC4, 0:NC4], pi == 0, pi == 1, [part, self.ident], [P[3]])
                fw.op(fw.dve, lambda: nc.vector.tensor_copy(out=rows[0:1, 0, 0:NC4], in_=P[3][0:1, 0:NC4]), [P[3]], [rows])
                self.ts(fw.dve, rows[0:1, 1, 0:NC4], P[1][0:1, 0:NC4], -1.0, None, ALU.mult, None, [P[1]], [rows])
                for h in range(H):
                    fw.op(fw.dve, lambda: nc.vector.tensor_tensor_scan(out=rows[0:1, 2, h:NC4:4], data0=rows[0:1, 0, h:NC4:4],
                                                                      data1=rows[0:1, 1, h:NC4:4], initial=0.0, op0=ALU.max, op1=ALU.add),
                          [rows], [rows])
                self.tt(fw.dve, rows[0:1, 3, 0:NC4], rows[0:1, 2, 0:NC4], rows[0:1, 1, 0:NC4], ALU.subtract, [rows], [rows])
                fw.op(fw.dve, lambda: nc.vector.memset(rows[0:1, 4, 0:4], 0.0), [], [rows])
                if NC4 > 4:
                    fw.op(fw.dve, lambda: nc.vector.tensor_copy(out=rows[0:1, 4, 4:NC4], in_=rows[0:1, 2, 0:NC4 - 4]), [rows], [rows])
                self.tt(fw.dve, rows[0:1, 5, 0:NC4], rows[0:1, 4, 0:NC4], rows[0:1, 3, 0:NC4], ALU.subtract, [rows], [rows])
                self.split2(rows[0:1, 3, 0:NC4], [rows], rwh, rwl, rwt, hi_ap=rwh[0:1, 0, 0:NC4], lo_ap=rwl[0:1, 0, 0:NC4], tmp_ap=rwt[0:1, 0, 0:NC4])
                self.split2(rows[0:1, 5, 0:NC4], [rows], rwh, rwl, rwt, hi_ap=rwh[0:1, 1, 0:NC4], lo_ap=rwl[0:1, 1, 0:NC4], tmp_ap=rwt[0:1, 1, 0:NC4])
                for pi, part in enumerate((rwh, rwl)):
                    self.mm(P[4][:, 0:NC4], onesb2[0:1, :], part[0:1, 0, 0:NC4], pi == 0, pi == 1, [onesb2, part], [P[4]])
                for pi, part in enumerate((rwh, rwl)):
                    self.mm(P[5][:, 0:NC4], onesb2[0:1, :], part[0:1, 1, 0:NC4], pi == 0, pi == 1, [onesb2, part], [P[5]])
                self.tt(fw.dve, tmpk[:], A[:], P[4][:, 0:NC4], ALU.subtract, [A, P[4]], [tmpk])
                self.actf(KS[:], tmpk[:], AF.Exp, [tmpk], [KS])
                self.ts(fw.dve, KS[:], KS[:], float(DH ** -0.5), None, ALU.mult, None, [KS], [KS])
                self.tt(fw.dve, tmpk[:], Bn[:], P[4][:, 0:NC4], ALU.subtract, [Bn, P[4]], [tmpk])
                self.actf(RS[:], tmpk[:], AF.Exp, [tmpk], [RS])
                self.actf(SC[:], P[5][:, 0:NC4], AF.Exp, [P[5]], [SC])
                fw.barrier()
            with contextlib.ExitStack() as st:
                wout = fw.sb([128, 16, D], BF16, "wout", st)
                self.load_w_bf16(wout, w_out, 2048, D)
                gnbc = fw.sb([128, 2048], F32, "gnbc", st)
                skbc = fw.sb([128, 2048], F32, "skbc", st)
                fw.dma(fw.sp, gnbc[:], gn_d[:, :], w=[gnbc])
                fw.dma(fw.sp, skbc[:], sk_d[:, :], w=[skbc])
                qk = [fw.sb([128, 32, 128], BF16, "qk%d" % i, st) for i in range(2)]
                vt = [fw.sb([128, 2048], BF16, "vt%d" % i, st) for i in range(2)]
                ct = [fw.sb([128, 2048], BF16, "ct%d" % i, st) for i in range(2)]
                zt = [fw.sb([128, 2048], BF16, "zt%d" % i, st) for i in range(2)]
                skc = [fw.sb([128, 2048], F32, "skc%d" % i, st) for i in range(2)]
                Cst = [fw.sb([128, 4, DH], F32, "C%d" % h, st) for h in range(H)]
                Cbf = [fw.sb([128, 4, DH], BF16, "Cbf%d" % h, st) for h in range(H)]
                nst = [fw.sb([128, 4], F32, "n%d" % h, st) for h in range(H)]
                nbf = [fw.sb([128, 4], BF16, "nbf%d" % h, st) for h in range(H)]
                kd = [fw.sb([128, DH], BF16, "kd%d" % i, st) for i in range(2)]
                sT = [fw.sb([128, 128], BF16, "sT%d" % i, st) for i in range(2)]
                hh = [fw.sb([128, DH], F32, "hh%d" % i, st) for i in range(2)]
                t2 = [fw.sb([128, DH], F32, "t2%d" % i, st) for i in range(2)]
                sq = fw.sb([128, DH], BF16, "sq", st)
                sm = [[fw.sb([128, 1], F32, "sm", st) for _ in range(7)] for _ in range(2)]
                yb = [fw.sb([128, 2048], BF16, "yb%d" % i, st) for i in range(2)]
                yT = [fw.sb([128, 16, 128], BF16, "yT%d" % i, st) for i in range(2)]
                hb = [fw.sb([128, D], F32, "hb%d" % i, st) for i in range(2)]
                for h in range(H):
                    fw.op(fw.pool, lambda: nc.gpsimd.memset(Cst[h][:], 0.0), [], [Cst[h]])
                    fw.op(fw.pool, lambda: nc.gpsimd.memset(Cbf[h][:], 0.0), [], [Cbf[h]])
                    fw.op(fw.pool, lambda: nc.gpsimd.memset(nst[h][:], 0.0), [], [nst[h]])
                    fw.op(fw.pool, lambda: nc.gpsimd.memset(nbf[h][:], 0.0), [], [nbf[h]])
                src = self.src_tiles()
                n = 0
                for c in range(NT):
                    q_ = qk[c % 2]
                    fw.dma(fw.sp, q_[:], qk_d[:, :, c * 128:(c + 1) * 128].rearrange("c p t -> p c t"), w=[q_])
                    v_, c_, z_, sk_ = vt[c % 2], ct[c % 2], zt[c % 2], skc[c % 2]
                    fw.dma(fw.sp, v_[:], v_d[c * 128:(c + 1) * 128, :], w=[v_])
                    fw.dma(fw.sp, c_[:], c_d[c * 128:(c + 1) * 128, :], w=[c_])
                    fw.dma(fw.sp, z_[:], zs_d[c * 128:(c + 1) * 128, :], w=[z_])
                    fw.dma(fw.sp, hb[c % 2][:], src[c].t, r=[src[c]], w=[hb[c % 2]])
                    self.tt(fw.pool, sk_[:], c_[:], skbc[:], ALU.mult, [c_, skbc], [sk_])
                    y_ = yb[c % 2]
                    for h in range(H):
                        i = n % 2
                        n += 1
                        col = c * 4 + h
                        s1, nm, ss, std, rstd, dd, rd = sm[i]
                        for jj in range(4):
                            self.mm(P[0][:, jj * 128:(jj + 1) * 128], q_[:, 16 + 4 * h + jj, :], self.ident[:], True, True,
                                    [q_, self.ident], [P[0]])
                        self.actf(kd[i][:], P[0][:, :], AF.Copy, [P[0], KS], [kd[i]], scale=KS[:, col:col + 1])
                        for jj in range(4):
                            self.mm(P[1][:, 0:128], q_[:, 16 + 4 * h + jj, :], q_[:, 4 * h + jj, :], jj == 0, jj == 3, [q_], [P[1]])
                        self.stt(fw.dve, sT[i][:], P[1][:, 0:128], KS[:, col:col + 1], tri[:], ALU.mult, ALU.mult, [P[1], KS, tri], [sT[i]])
                        self.mm(P[2][:, :], sT[i][:], v_[:, h * DH:(h + 1) * DH], True, False, [sT[i], v_], [P[2]])
                        for jj in range(4):
                            self.mm(P[2][:, :], q_[:, 4 * h + jj, :], Cbf[h][:, jj, :], False, jj == 3, [q_, Cbf[h]], [P[2]])
                        self.mm(P[3][:, 0:1], sT[i][:], onesb[:], True, False, [sT[i], onesb], [P[3]])
                        for jj in range(4):
                            self.mm(P[3][:, 0:1], q_[:, 4 * h + jj, :], nbf[h][:, jj:jj + 1], False, jj == 3, [q_, nbf[h]], [P[3]])
                        self.actf(dd[:], P[3][:, 0:1], AF.Abs, [P[3]], [dd])
                        self.tt(fw.dve, dd[:], dd[:], RS[:, col:col + 1], ALU.max, [dd, RS], [dd])
                        fw.op(fw.dve, lambda: nc.vector.reciprocal(out=rd[:], in_=dd[:]), [dd], [rd])
                        self.actf(hh[i][:], P[2][:, :], AF.Copy, [P[2], rd], [hh[i], s1], scale=rd[:], accum_out=s1[:])
                        self.ts(fw.dve, nm[:], s1[:], -1.0 / DH, None, ALU.mult, None, [s1], [nm])
                        self.actf(sq[:], hh[i][:], AF.Square, [hh[i], nm], [sq, ss], bias=nm[:], accum_out=ss[:])
                        self.rstd_from_ss(ss, std, rstd, DH, 1e-6)
                        hs = slice(h * DH, (h + 1) * DH)
                        self.stt(fw.pool, t2[i][:], hh[i][:], nm[:], gnbc[:, hs], ALU.add, ALU.mult, [hh[i], nm, gnbc], [t2[i]])
                        self.stt(fw.dve, t2[i][:], t2[i][:], rstd[:], sk_[:, hs], ALU.mult, ALU.add, [t2[i], rstd, sk_], [t2[i]])
                        self.tt(fw.pool, y_[:, hs], t2[i][:], z_[:, hs], ALU.mult, [t2[i], z_], [y_])
                        for jj in range(4):
                            self.mm(P[3][:, 1 + jj:2 + jj], kd[i][:, jj * 128:(jj + 1) * 128], onesb[:], True, True, [kd[i], onesb], [P[3]])
                        self.stt(fw.dve, nst[h][:], nst[h][:], SC[:, col:col + 1], P[3][:, 1:5], ALU.mult, ALU.add, [nst[h], SC, P[3]], [nst[h]])
                        for jj in range(4):
                            pb = P[4 + (jj % 2)]
                            self.mm(pb[:, :], kd[i][:, jj * 128:(jj + 1) * 128], v_[:, hs], True, True, [kd[i], v_], [pb])
                            self.stt(fw.dve, Cst[h][:, jj, :], Cst[h][:, jj, :], SC[:, col:col + 1], pb[:, :], ALU.mult, ALU.add,
                                     [Cst[h], SC, pb], [Cst[h]])
                        if c + 1 < NT:
                            ncol = (c + 1) * 4 + h
                            self.actf(Cbf[h][:], Cst[h][:], AF.Copy, [Cst[h], SC], [Cbf[h]], scale=SC[:, ncol:ncol + 1])
                            self.ts(fw.pool, nbf[h][:], nst[h][:], SC[:, ncol:ncol + 1], None, ALU.mult, None, [nst[h], SC], [nbf[h]])
                    self.outproj(y_, yT[c % 2], wout, hb[c % 2], c)
                fw.barrier()
        self.first = False


    def rwkv(self, li, j):
        fw = self.fw
        nc = self.nc
        S = self.S
        NT = self.NT
        P = self.P
        H, DH = 16, 64
        wrkv_d = self.inp("rw_w_rkv_%d" % j, [3 * D, D])
        wout_d = self.inp("rw_w_out_%d" % j, [D, D])
        la_d = self.inp("rw_lora_a_%d" % j, [D, 288])
        lbw_d = self.inp("rw_w_lora_b_%d" % j, [64, D])
        lba_d = self.inp("rw_a_lora_b_%d" % j, [64, D])
        lbg_d = self.inp("rw_g_lora_b_%d" % j, [160, D])
        mu_d = self.inp("rw_muT_%d" % j, [128, 48])
        vec_d = self.inp("rw_vec_bc_%d" % j, [128, 7 * D])
        msk_d = self.inp("c_rw_masks", [128, 4 * 512])
        mx_d = self.inp("c_rw_mx", [128, 3 * 128])
        if not hasattr(self, "rw_scr"):
            self.rw_scr = (fw.dram("rw_rkv", [3, S, D], BF16), fw.dram("rw_logw", [S, D], F32),
                           fw.dram("rw_ag", [2, S, D], BF16))
        rkv_d, lw_d, ag_d = self.rw_scr
        with contextlib.ExitStack() as st:
            wrkv = fw.sb([128, 24, D], BF16, "wrkv", st)
            lora_a = fw.sb([128, 8, 288], BF16, "lora_a", st)
            lbw = fw.sb([64, D], BF16, "lbw", st)
            lba = fw.sb([64, D], BF16, "lba", st)
            lbg0 = fw.sb([128, D], BF16, "lbg0", st)
            lbg1 = fw.sb([32, D], BF16, "lbg1", st)
            mu = fw.sb([128, 8, 6], F32, "mu", st)
            vec = fw.sb([128, 2, D], F32, "vec", st)
            gbc = fw.sb([128, D], F32, "gbc", st)
            hb = [fw.sb([128, D], F32, "hb%d" % i, st) for i in range(2)]
            xn = [fw.sb([128, D], BF16, "xn%d" % i, st) for i in range(2)]
            sq = fw.sb([128, D], BF16, "sq", st)
            stt_ = [[fw.sb([128, 1], F32, "st", st) for _ in range(3)] for _ in range(2)]
            xnT = [fw.sb([128, 8, 129], BF16, "xnT%d" % i, st) for i in range(2)]
            dd = fw.sb([128, 8, 128], BF16, "dd", st)
            mix = [fw.sb([128, 8, 128], BF16, "mix%d" % i, st) for i in range(3)]
            l1w = fw.sb([64, 128], BF16, "l1w", st)
            l1a = fw.sb([64, 128], BF16, "l1a", st)
            l1g0 = fw.sb([128, 128], BF16, "l1g0", st)
            l1g1 = fw.sb([32, 128], BF16, "l1g1", st)
            ob = [fw.sb([128, D], BF16, "ob%d" % i, st) for i in range(4)]
            of = [fw.sb([128, D], F32, "of%d" % i, st) for i in range(2)]
            g_d = self.inp("norm_mix_bc_%d" % li, [128, D])
            fw.dma(fw.sp, gbc[:], g_d[:, :], w=[gbc])
            fw.dma(fw.sp, mu[:].rearrange("p a b -> p (a b)"), mu_d[:, :], w=[mu])
            fw.dma(fw.sp, vec[:].rearrange("p a b -> p (a b)"), vec_d[:, 0:2 * D], w=[vec])
            self.load_w_bf16(wrkv, wrkv_d, 3 * D, D)
            self.load_w_bf16(lora_a, la_d, D, 288)
            fw.dma(fw.pool, lbw[:], lbw_d[:, :], w=[lbw])
            fw.dma(fw.pool, lba[:], lba_d[:, :], w=[lba])
            fw.dma(fw.pool, lbg0[:], lbg_d[0:128, :], w=[lbg0])
            fw.dma(fw.pool, lbg1[:], lbg_d[128:160, :], w=[lbg1])
            fw.op(fw.pool, lambda: nc.gpsimd.memset(xnT[1][:, :, 128:129], 0.0), [], [xnT[1]])
            src = self.src_tiles()
            nob = 0
            for c in range(NT):
                xT = xnT[c % 2]
                xTp = xnT[(c + 1) % 2]
                self.norm_tile(src[c], gbc, hb[c % 2], xn[c % 2], stt_[c % 2], sq)
                fw.op(fw.pool, lambda: nc.gpsimd.tensor_copy(out=xT[:, :, 0:1], in_=xTp[:, :, 128:129]), [xTp], [xT])
                self.transpose_to(xn[c % 2], xT, 1, 8, [P[6], P[7]], [fw.act, fw.dve])
                self.tt(fw.pool, dd[:], xT[:, :, 0:128], xT[:, :, 1:129], ALU.subtract, [xT], [dd])
                for i in range(6):
                    mx = mix[i % 3]
                    for k in range(8):
                        self.stt(fw.dve, mx[:, k, :], dd[:, k, :], mu[:, k, i:i + 1], xT[:, k, 1:129], ALU.mult, ALU.add, [dd, mu, xT], [mx])
                    if i < 3:
                        o_ = ob[nob % 4]
                        nob += 1
                        for half in range(2):
                            pb = P[(2 * i + half) % 4]
                            for k in range(8):
                                self.mm(pb[:, :], mx[:, k, :], wrkv[:, i * 8 + k, half * 512:(half + 1) * 512], k == 0, k == 7, [mx, wrkv], [pb])
                            if half == 0:
                                self.actf(o_[:, 0:512], pb[:, :], AF.Copy, [pb], [o_])
                            else:
                                fw.op(fw.dve, lambda: nc.vector.tensor_copy(out=o_[:, 512:1024], in_=pb[:, :]), [pb], [o_])
                        fw.dma(fw.sp, rkv_d[i, c * 128:(c + 1) * 128, :], o_[:], r=[o_])
                    elif i == 3:
                        for k in range(8):
                            self.mm(P[4][0:64, 0:128], lora_a[:, k, 0:64], mx[:, k, :], k == 0, k == 7, [lora_a, mx], [P[4]])
                        self.actf(l1w[:], P[4][0:64, 0:128], AF.Tanh, [P[4]], [l1w])
                        o_ = of[c % 2]
                        for half in range(2):
                            pb = P[half]
                            self.mm(pb[:, :], l1w[:], lbw[:, half * 512:(half + 1) * 512], True, True, [l1w, lbw], [pb])
                            self.tt(fw.dve, o_[:, half * 512:(half + 1) * 512], pb[:, :], vec[:, 0, half * 512:(half + 1) * 512], ALU.add, [pb, vec], [o_])
                        self.actf(o_[:], o_[:], AF.Sigmoid, [o_], [o_])
                        self.ts(fw.pool, o_[:], o_[:], -0.6065306597126334, None, ALU.mult, None, [o_], [o_])
                        fw.dma(fw.sp, lw_d[c * 128:(c + 1) * 128, :], o_[:], r=[o_])
                    elif i == 4:
                        for k in range(8):
                            self.mm(P[4][0:64, 128:256], lora_a[:, k, 64:128], mx[:, k, :], k == 0, k == 7, [lora_a, mx], [P[4]])
                        self.actf(l1a[:], P[4][0:64, 128:256], AF.Copy, [P[4]], [l1a])
                        o_ = ob[nob % 4]
                        nob += 1
                        for half in range(2):
                            pb = P[2 + half]
                            self.mm(pb[:, :], l1a[:], lba[:, half * 512:(half + 1) * 512], True, True, [l1a, lba], [pb])
                            self.tt(fw.dve, sq[:, half * 512:(half + 1) * 512], pb[:, :], vec[:, 1, half * 512:(half + 1) * 512], ALU.add, [pb, vec], [sq])
                        self.actf(o_[:], sq[:], AF.Sigmoid, [sq], [o_])
                        fw.dma(fw.sp, ag_d[0, c * 128:(c + 1) * 128, :], o_[:], r=[o_])
                    else:
                        for k in range(8):
                            self.mm(P[5][:, 0:128], lora_a[:, k, 128:256], mx[:, k, :], k == 0, k == 7, [lora_a, mx], [P[5]])
                        for k in range(8):
                            self.mm(P[5][0:32, 128:256], lora_a[:, k, 256:288], mx[:, k, :], k == 0, k == 7, [lora_a, mx], [P[5]])
                        self.actf(l1g0[:], P[5][:, 0:128], AF.Sigmoid, [P[5]], [l1g0])
                        self.actf(l1g1[:], P[5][0:32, 128:256], AF.Sigmoid, [P[5]], [l1g1])
                        o_ = ob[nob % 4]
                        nob += 1
                        for half in range(2):
                            pb = P[half]
                            self.mm(pb[:, :], l1g0[:], lbg0[:, half * 512:(half + 1) * 512], True, False, [l1g0, lbg0], [pb])
                            self.mm(pb[:, :], l1g1[:], lbg1[:, half * 512:(half + 1) * 512], False, True, [l1g1, lbg1], [pb])
                            if half == 0:
                                self.actf(o_[:, 0:512], pb[:, :], AF.Copy, [pb], [o_])
                            else:
                                fw.op(fw.dve, lambda: nc.vector.tensor_copy(out=o_[:, 512:1024], in_=pb[:, :]), [pb], [o_])
                        fw.dma(fw.sp, ag_d[1, c * 128:(c + 1) * 128, :], o_[:], r=[o_])
            fw.barrier()
        with contextlib.ExitStack() as st:
            wout = fw.sb([128, 8, D], BF16, "wout", st)
            self.load_w_bf16(wout, wout_d, D, D)
            vec = fw.sb([128, 5, D], F32, "vec", st)
            fw.dma(fw.sp, vec[:].rearrange("p a b -> p (a b)"), vec_d[:, 2 * D:7 * D], w=[vec])
            msk = fw.sb([128, 4, 512], F32, "msk", st)
            fw.dma(fw.sp, msk[:].rearrange("p a b -> p (a b)"), msk_d[:, :], w=[msk])
            mxb = fw.sb([128, 4, 128], BF16, "mxb", st)
            fw.dma(fw.pool, mxb[:, 0:3, :].rearrange("p a b -> p (a b)"), mx_d[:, :], w=[mxb])
            fw.op(fw.pool, lambda: nc.gpsimd.memset(mxb[:, 3, :], 1.0), [], [mxb])
            lwh = fw.sb([128, D], BF16, "lwh", st)
            lwl = fw.sb([128, D], BF16, "lwl", st)
            rb = [fw.sb([128, D], BF16, "rb%d" % i, st) for i in range(2)]
            kb = [fw.sb([128, D], BF16, "kb%d" % i, st) for i in range(2)]
            vb = [fw.sb([128, D], BF16, "vb%d" % i, st) for i in range(2)]
            al = [fw.sb([128, D], BF16, "al%d" % i, st) for i in range(2)]
            gb = [fw.sb([128, D], BF16, "gb%d" % i, st) for i in range(2)]
            lw = [fw.sb([128, D], F32, "lw%d" % i, st) for i in range(2)]
            hb = [fw.sb([128, D], F32, "hb%d" % i, st) for i in range(2)]
            T = [fw.sb([128, D], F32, "T%d" % i, st) for i in range(7)]
            Xtm = [fw.sb([128, D], BF16, "Xtm%d" % i, st) for i in range(4)]
            kh = fw.sb([128, D], BF16, "kh", st)
            bh = fw.sb([128, D], BF16, "bh", st)
            XT = [fw.sb([64, 16, 128], BF16, "XT%d" % i, st) for i in range(4)]
            Agg = [[fw.sb([128, 4, 128], BF16, "Ag%d" % i, st) for i in range(2)] for _ in range(4)]
            Aak = fw.sb([128, 16, 128], BF16, "Aak", st)
            RBt = fw.sb([128, 16, 128], BF16, "RBt", st)
            RKt = fw.sb([128, 16, 128], BF16, "RKt", st)
            Wall = fw.sb([128, 16, 128], BF16, "Wall", st)
            Xsg = [[fw.sb([128, 4, 128], BF16, "Xs%d" % i, st) for i in range(2)] for _ in range(4)]
            XTsg = [[fw.sb([128, 4, 128], BF16, "XTs%d" % i, st) for i in range(2)] for _ in range(4)]
            Wsg = [[fw.sb([128, 4, 128], BF16, "Ws%d" % i, st) for i in range(2)] for _ in range(4)]
            Mst = fw.sb([64, 16, 64], F32, "Mst", st)
            Mbf = fw.sb([64, 16, 64], BF16, "Mbf", st)
            pmpl = fw.sb([64, 32], F32, "pmpl", st)
            RHSb = fw.sb([128, D], BF16, "RHSb", st)
            Ub = fw.sb([128, D], BF16, "Ub", st)
            s16 = [fw.sb([128, 16], F32, "s16_%d" % i, st) for i in range(6)]
            yb = [fw.sb([128, D], BF16, "yb%d" % i, st) for i in range(2)]
            yT = [fw.sb([128, 8, 128], BF16, "yT%d" % i, st) for i in range(2)]
            fw.op(fw.pool, lambda: nc.gpsimd.memset(Mst[:], 0.0), [], [Mst])
            src = self.src_tiles()

            def v3(t):
                return t.rearrange("p (h d) -> p h d", h=16)

            def bc16(t):
                return t[:].unsqueeze(2).to_broadcast([128, 16, 64])

            for c in range(NT):
                i2 = c % 2
                rows = slice(c * 128, (c + 1) * 128)
                r_, k_, v_, a_, g_, lw_ = rb[i2], kb[i2], vb[i2], al[i2], gb[i2], lw[i2]
                fw.dma(fw.sp, r_[:], rkv_d[0, rows, :], w=[r_])
                fw.dma(fw.sp, k_[:], rkv_d[1, rows, :], w=[k_])
                fw.dma(fw.sp, v_[:], rkv_d[2, rows, :], w=[v_])
                fw.dma(fw.sp, lw_[:], lw_d[rows, :], w=[lw_])
                fw.dma(fw.sp, a_[:], ag_d[0, rows, :], w=[a_])
                fw.dma(fw.sp, g_[:], ag_d[1, rows, :], w=[g_])
                fw.dma(fw.sp, hb[i2][:], src[c].t, r=[src[c]], w=[hb[i2]])
                T1, T2, T3, T4, T5, T6, T7 = T
                ss16, sd16, rn16, bon16, m16, rs16 = s16
                self.tt(fw.pool, T5[:], k_[:], vec[:, 0, :], ALU.mult, [k_, vec], [T5])
                self.tt(fw.pool, T6[:], T5[:], T5[:], ALU.mult, [T5], [T6])
                fw.op(fw.dve, lambda: nc.vector.tensor_reduce(out=ss16[:], in_=v3(T6[:]), axis=AX.X, op=ALU.add), [T6], [ss16])
                self.ts(fw.dve, ss16[:], ss16[:], 1e-24, None, ALU.max, None, [ss16], [ss16])
                self.actf(sd16[:], ss16[:], AF.Sqrt, [ss16], [sd16])
                fw.op(fw.dve, lambda: nc.vector.reciprocal(out=rn16[:], in_=sd16[:]), [sd16], [rn16])
                self.tt(fw.dve, v3(T5[:]), v3(T5[:]), bc16(rn16), ALU.mult, [T5, rn16], [T5])
                self.stt(fw.dve, T6[:], a_[:], -1.0, vec[:, 1, :], ALU.add, ALU.mult, [a_, vec], [T6])
                self.stt(fw.dve, T2[:], T6[:], 1.0, k_[:], ALU.add, ALU.mult, [T6, k_], [T2])
                self.tt(fw.pool, T6[:], r_[:], T2[:], ALU.mult, [r_, T2], [T6])
                self.tt(fw.pool, T6[:], T6[:], vec[:, 2, :], ALU.mult, [T6, vec], [T6])
                fw.op(fw.dve, lambda: nc.vector.tensor_reduce(out=bon16[:], in_=v3(T6[:]), axis=AX.X, op=ALU.add), [T6], [bon16])
                self.tt(fw.pool, T6[:], T5[:], a_[:], ALU.mult, [T5, a_], [T6])
                self.split2(lw_[:], [lw_], lwh, lwl, T7)
                for half in range(2):
                    hs = slice(half * 512, (half + 1) * 512)
                    for pi, part in enumerate((lwh, lwl)):
                        self.mm(P[half][:, :], mxb[:, 0, :], part[:, hs], pi == 0, pi == 1, [mxb, part], [P[half]])
                    for pi, part in enumerate((lwh, lwl)):
                        self.mm(P[2 + half][:, :], mxb[:, 1, :], part[:, hs], pi == 0, pi == 1, [mxb, part], [P[2 + half]])
                for h in range(H):
                    for pi, part in enumerate((lwh, lwl)):
                        self.mm(P[4][0:64, h:h + 1], part[:, h * 64:(h + 1) * 64], mxb[:, 2, 0:1], pi == 0, pi == 1, [part, mxb], [P[4]])
                for h in range(H):
                    for pi, part in enumerate((lwh, lwl)):
                        self.mm(P[4][0:64, 16 + h:17 + h], part[:, h * 64:(h + 1) * 64], mxb[:, 3, 0:1], pi == 0, pi == 1, [part, mxb], [P[4]])
                self.actf(pmpl[:], P[4][0:64, 0:32], AF.Exp, [P[4]], [pmpl])
                for half in range(2):
                    hs = slice(half * 512, (half + 1) * 512)
                    self.actf(T4[:, hs], P[half][:, :], AF.Exp, [P[half]], [T4])
                    self.actf(T1[:, hs], P[half][:, :], AF.Exp, [P[half]], [T1], scale=-1.0)
                    self.tt(fw.dve, T3[:, hs], P[half][:, :], lw_[:, hs], ALU.subtract, [P[half], lw_], [T3])
                    self.actf(T7[:, hs], P[2 + half][:, :], AF.Exp, [P[2 + half]], [T7])
                self.actf(T3[:], T3[:], AF.Exp, [T3], [T3])
                self.tt(fw.pool, Xtm[0][:], r_[:], T4[:], ALU.mult, [r_, T4], [Xtm[0]])
                self.tt(fw.dve, Xtm[1][:], T2[:], T1[:], ALU.mult, [T2, T1], [Xtm[1]])
                self.tt(fw.pool, Xtm[2][:], T6[:], T1[:], ALU.mult, [T6, T1], [Xtm[2]])
                self.stt(fw.dve, Xtm[3][:], T5[:], -1.0, T3[:], ALU.mult, ALU.mult, [T5, T3], [Xtm[3]])
                self.tt(fw.dve, kh[:], T2[:], T7[:], ALU.mult, [T2, T7], [kh])
                self.tt(fw.pool, bh[:], T6[:], T7[:], ALU.mult, [T6, T7], [bh])
                self.tt(fw.dve, Mbf[:], Mst[:], pmpl[:, 0:16].unsqueeze(2).to_broadcast([64, 16, 64]), ALU.mult, [Mst, pmpl], [Mbf])
                n = 0
                for q in range(4):
                    for g in range(4):
                        pb = P[4 + (n % 4)]
                        for jj in range(4):
                            h = g * 4 + jj
                            self.mm(pb[0:64, jj * 128:(jj + 1) * 128], Xtm[q][:, h * 64:(h + 1) * 64], self.ident[:], True, True,
                                    [Xtm[q], self.ident], [pb])
                        dst = XT[q][:, g * 4:(g + 1) * 4, :]
                        srcp = pb[0:64, :].rearrange("p (a b) -> p a b", a=4)
                        if n % 2 == 0:
                            self.actf(dst, srcp, AF.Copy, [pb], [XT[q]])
                        else:
                            fw.op(fw.dve, lambda: nc.vector.tensor_copy(out=dst, in_=srcp), [pb], [XT[q]])
                        n += 1
                rT, kT, bT, aT = XT
                cur = []
                for g in range(4):
                    Ag = Agg[g]
                    specs = ((aT, bT, 0, Ag[0], None), (bT, aT, 1, Ag[1], None), (kT, aT, 1, Aak, g), (bT, rT, 2, RBt, g), (kT, rT, 2, RKt, g))
                    for si, (lt, rt, mi, dstT, gg) in enumerate(specs):
                        pb = P[(g * 5 + si) % 8]
                        for jj in range(4):
                            h = g * 4 + jj
                            self.mm(pb[:, jj * 128:(jj + 1) * 128], lt[:, h, :], rt[:, h, :], True, True, [lt, rt], [pb])
                        dst = dstT[:].rearrange("p a b -> p (a b)") if gg is None else dstT[:, g * 4:(g + 1) * 4, :].rearrange("p a b -> p (a b)")
                        if si % 2 == 0:
                            self.tt(fw.dve, dst, pb[:, :], msk[:, mi, :], ALU.mult, [pb, msk], [dstT])
                        else:
                            self.tt(fw.dve, dst, pb[:, :], msk[:, mi, :], ALU.mult, [pb, msk], [dstT])
                    W = Wsg[g][0]
                    self.tt(fw.pool, W[:].rearrange("p a b -> p (a b)"), Ag[1][:].rearrange("p a b -> p (a b)"), msk[:, 3, :], ALU.add, [Ag[1], msk], [W])
                    cur.append([Ag[0], Ag[1], W])
                for lvl in range(1, 7):
                    for g in range(4):
                        X, XTt, W = cur[g]
                        Xn, XTn, Wn = Xsg[g][lvl % 2], XTsg[g][lvl % 2], Wsg[g][lvl % 2]
                        pX, pXT = P[2 * g], P[2 * g + 1]
                        for jj in range(4):
                            self.mm(pX[:, jj * 128:(jj + 1) * 128], XTt[:, jj, :], X[:, jj, :], True, True, [XTt, X], [pX])
                        self.actf(Xn[:].rearrange("p a b -> p (a b)"), pX[:, :], AF.Copy, [pX], [Xn])
                        if lvl < 6:
                            for jj in range(4):
                                self.mm(pXT[:, jj * 128:(jj + 1) * 128], X[:, jj, :], XTt[:, jj, :], True, True, [XTt, X], [pXT])
                            if g % 2 == 0:
                                self.actf(XTn[:].rearrange("p a b -> p (a b)"), pXT[:, :], AF.Copy, [pXT], [XTn])
                            else:
                                fw.op(fw.dve, lambda: nc.vector.tensor_copy(out=XTn[:].rearrange("p a b -> p (a b)"), in_=pXT[:, :]), [pXT], [XTn])
                        cur[g] = [Xn, XTn, W]
                    for g in range(4):
                        Xn, XTn, W = cur[g]
                        Wn = Wsg[g][lvl % 2]
                        pW = P[2 * g]
                        for jj in range(4):
                            self.mm(pW[:, jj * 128:(jj + 1) * 128], Xn[:, jj, :], W[:, jj, :], True, True, [Xn, W], [pW])
                        if lvl < 6:
                            self.tt(fw.dve, Wn[:].rearrange("p a b -> p (a b)"), W[:].rearrange("p a b -> p (a b)"), pW[:, :], ALU.add, [W, pW], [Wn])
                        else:
                            self.tt(fw.dve, Wall[:, g * 4:(g + 1) * 4, :].rearrange("p a b -> p (a b)"), W[:].rearrange("p a b -> p (a b)"), pW[:, :], ALU.add,
                                    [W, pW], [Wall])
                        cur[g] = [Xn, XTn, Wn]
                for h in range(H):
                    pb = P[h // 8]
                    cs = slice((h % 8) * 64, (h % 8) * 64 + 64)
                    hs = slice(h * 64, (h + 1) * 64)
                    self.mm(pb[:, cs], aT[:, h, :], Mbf[:, h, :], True, False, [aT, Mbf], [pb])
                    self.mm(pb[:, cs], Aak[:, h, :], v_[:, hs], False, True, [Aak, v_], [pb])
                self.actf(RHSb[:, 0:512], P[0][:, :], AF.Copy, [P[0]], [RHSb])
                fw.op(fw.dve, lambda: nc.vector.tensor_copy(out=RHSb[:, 512:1024], in_=P[1][:, :]), [P[1]], [RHSb])
                for h in range(H):
                    pb = P[2 + h // 8]
                    cs = slice((h % 8) * 64, (h % 8) * 64 + 64)
                    hs = slice(h * 64, (h + 1) * 64)
                    self.mm(pb[:, cs], Wall[:, h, :], RHSb[:, hs], True, True, [Wall, RHSb], [pb])
                self.actf(Ub[:, 0:512], P[2][:, :], AF.Copy, [P[2]], [Ub])
                fw.op(fw.dve, lambda: nc.vector.tensor_copy(out=Ub[:, 512:1024], in_=P[3][:, :]), [P[3]], [Ub])
                for h in range(H):
                    pb = P[6 + h // 8]
                    cs = slice((h % 8) * 64, (h % 8) * 64 + 64)
                    hs = slice(h * 64, (h + 1) * 64)
                    self.mm(pb[0:64, cs], bh[:, hs], Ub[:, hs], True, False, [bh, Ub], [pb])
                    self.mm(pb[0:64, cs], kh[:, hs], v_[:, hs], False, True, [kh, v_], [pb])
                for h in range(H):
                    pb = P[4 + h // 8]
                    cs = slice((h % 8) * 64, (h % 8) * 64 + 64)
                    hs = slice(h * 64, (h + 1) * 64)
                    self.mm(pb[:, cs], rT[:, h, :], Mbf[:, h, :], True, False, [rT, Mbf], [pb])
                    self.mm(pb[:, cs], RBt[:, h, :], Ub[:, hs], False, False, [RBt, Ub], [pb])
                    self.mm(pb[:, cs], RKt[:, h, :], v_[:, hs], False, True, [RKt, v_], [pb])
                self.tt(fw.dve, Mst[:], Mst[:], pmpl[:, 16:32].unsqueeze(2).to_broadcast([64, 16, 64]), ALU.mult, [Mst, pmpl], [Mst])
                M2 = Mst[:].rearrange("p a b -> p (a b)")
                self.tt(fw.dve, M2[:, 0:512], M2[:, 0:512], P[6][0:64, :], ALU.add, [Mst, P[6]], [Mst])
                self.tt(fw.dve, M2[:, 512:1024], M2[:, 512:1024], P[7][0:64, :], ALU.add, [Mst, P[7]], [Mst])
                self.actf(T1[:, 0:512], P[4][:, :], AF.Copy, [P[4]], [T1])
                fw.op(fw.dve, lambda: nc.vector.tensor_copy(out=T1[:, 512:1024], in_=P[5][:, :]), [P[5]], [T1])
                fw.op(fw.dve, lambda: nc.vector.tensor_reduce(out=m16[:], in_=v3(T1[:]), axis=AX.X, op=ALU.add), [T1], [m16])
                self.ts(fw.dve, m16[:], m16[:], -1.0 / DH, None, ALU.mult, None, [m16], [m16])
                self.tt(fw.pool, v3(T1[:]), v3(T1[:]), bc16(m16), ALU.add, [T1, m16], [T1])
                self.tt(fw.pool, T4[:], T1[:], T1[:], ALU.mult, [T1], [T4])
                fw.op(fw.dve, lambda: nc.vector.tensor_reduce(out=ss16[:], in_=v3(T4[:]), axis=AX.X, op=ALU.add), [T4], [ss16])
                self.actf(sd16[:], ss16[:], AF.Sqrt, [ss16], [sd16], scale=1.0 / DH, bias=self.eps_tile(64e-5)[0:128, :])
                fw.op(fw.dve, lambda: nc.vector.reciprocal(out=rs16[:], in_=sd16[:]), [sd16], [rs16])
                self.tt(fw.dve, v3(T1[:]), v3(T1[:]), bc16(rs16), ALU.mult, [T1, rs16], [T1])
                self.tt(fw.pool, T1[:], T1[:], vec[:, 3, :], ALU.mult, [T1, vec], [T1])
                self.tt(fw.pool, T1[:], T1[:], vec[:, 4, :], ALU.add, [T1, vec], [T1])
                self.tt(fw.dve, v3(T4[:]), v3(v_[:]), bc16(bon16), ALU.mult, [v_, bon16], [T4])
                self.tt(fw.pool, T1[:], T1[:], T4[:], ALU.add, [T1, T4], [T1])
                self.tt(fw.dve, yb[i2][:], T1[:], g_[:], ALU.mult, [T1, g_], [yb[i2]])
                self.outproj(yb[i2], yT[i2], wout, hb[i2], c, nk=8)
            fw.barrier()
        self.first = False

    def host_extra(self, name, inputs):
        S = self.S
        f32 = np.float32
        if name.startswith("ret_w_in_"):
            return inputs["ret_w_in"][int(name[-1])]
        if name.startswith("ret_w_out_"):
            return inputs["ret_w_out"][int(name[-1])]
        if name.startswith("ret_gn_bc_"):
            return bc128(inputs["ret_gn"][int(name[-1])])
        if name in ("c_cosT", "c_sinT"):
            pos = np.arange(S, dtype=f32)
            inv = (f32(1.0) / (f32(10000.0) ** np.linspace(0.0, 1.0, 128, dtype=f32))).astype(f32)
            ang = (pos[:, None] * inv[None, :]).astype(f32)
            t = np.cos(ang.astype(np.float64)) if name == "c_cosT" else np.sin(ang.astype(np.float64))
            return np.ascontiguousarray(t.T.astype(f32))
        if name == "c_ret_maskT":
            lg = np.log1p(-2.0 ** (-5.0 - np.arange(4, dtype=np.float64)))
            idx = np.arange(128)
            rel = idx[None, :] - idx[:, None]
            m = np.where(rel[None] >= 0, np.exp(lg[:, None, None] * np.maximum(rel, 0)[None]), 0.0)
            return np.ascontiguousarray(m.transpose(1, 0, 2).reshape(128, 4 * 128).astype(f32))
        if name == "c_ret_dec":
            lg = np.log1p(-2.0 ** (-5.0 - np.arange(4, dtype=np.float64)))
            idx = np.arange(128, dtype=np.float64)
            kdec = np.exp(lg[None, :] * (127.0 - idx)[:, None])
            qdec = np.exp(lg[None, :] * (idx + 1.0)[:, None])
            return np.concatenate([kdec, qdec], axis=1).astype(f32)
        if name.startswith("ml_"):
            j = int(name.split("_")[-1])
            if name.startswith("ml_w_in_"):
                return inputs["ml_w_in"][j]
            if name.startswith("ml_w_out_"):
                return inputs["ml_w_out"][j]
            if name.startswith("ml_bd_"):
                out = np.zeros((128, 48, 128), f32)
                for wi, key in enumerate(("ml_wq", "ml_wk", "ml_wv")):
                    w = inputs[key][j]
                    for cc in range(16):
                        for n_ in range(32):
                            out[4 * n_:4 * n_ + 4, wi * 16 + cc, 4 * n_:4 * n_ + 4] = w[cc * 32 + n_]
                return out.reshape(128, 48 * 128)
            if name.startswith("ml_wg_"):
                w = inputs["ml_w_gate"][j]
                return np.ascontiguousarray(w.reshape(48, 128, 8).transpose(1, 0, 2).reshape(128, 48 * 8))
            if name.startswith("ml_cw_"):
                w = inputs["ml_conv_w"][j]
                return np.ascontiguousarray(w.reshape(4, 16, 128).transpose(2, 1, 0).reshape(128, 64))
            if name.startswith("ml_cb_"):
                return np.ascontiguousarray(inputs["ml_conv_b"][j].reshape(16, 128).T)
            if name.startswith("ml_bg_bc_"):
                return bc128(np.tile(inputs["ml_b_gate"][j], S // 128))
            if name.startswith("ml_gn_bc_"):
                return bc128(inputs["ml_gn"][j])
            if name.startswith("ml_skip_bc_"):
                return bc128(inputs["ml_skip"][j])
        if name.startswith("rw_"):
            j = int(name.split("_")[-1])
            if name.startswith("rw_w_rkv_"):
                return inputs["rw_w_rkv"][j].reshape(3 * 1024, 1024)
            if name.startswith("rw_w_out_"):
                return inputs["rw_w_out"][j]
            if name.startswith("rw_lora_a_"):
                return np.concatenate([inputs["rw_w_lora_a"][j], inputs["rw_a_lora_a"][j], inputs["rw_g_lora_a"][j]], axis=1)
            if name.startswith("rw_w_lora_b_"):
                return inputs["rw_w_lora_b"][j]
            if name.startswith("rw_a_lora_b_"):
                return inputs["rw_a_lora_b"][j]
            if name.startswith("rw_g_lora_b_"):
                return inputs["rw_g_lora_b"][j]
            if name.startswith("rw_muT_"):
                m = inputs["rw_mu"][j]
                return np.ascontiguousarray(m.reshape(6, 8, 128).transpose(2, 1, 0).reshape(128, 48))
            if name.startswith("rw_vec_bc_"):
                vs = [inputs["rw_w0"][j], inputs["rw_a0"][j], inputs["rw_k_k"][j], inputs["rw_k_a"][j],
                      inputs["rw_r_k"][j].reshape(-1), inputs["rw_gn_g"][j], inputs["rw_gn_b"][j]]
                return bc128(np.concatenate(vs))
        if name == "c_rw_masks":
            i = np.arange(128)
            m0 = (i[:, None] > i[None, :]).astype(f32)
            m1 = (i[:, None] < i[None, :]).astype(f32)
            m2 = (i[:, None] <= i[None, :]).astype(f32)
            m3 = np.eye(128, dtype=f32)
            return np.concatenate([np.tile(m, (1, 4)) for m in (m0, m1, m2, m3)], axis=1)
        if name == "c_rw_mx":
            i = np.arange(128)
            tri = (i[:, None] <= i[None, :]).astype(f32)
            selmid = np.broadcast_to((i[:, None] <= 63), (128, 128)).astype(f32)
            mx1 = tri - selmid
            mx2 = 1.0 - tri
            return np.concatenate([mx1, mx2, selmid], axis=1)
        if name == "c_tri":
            return np.triu(np.ones((128, 128), f32))
        if name == "c_identf":
            return np.eye(128, dtype=f32)
        raise KeyError(name)

    def ffn(self, li):
        fw = self.fw
        nc = self.nc
        S = self.S
        P = self.P
        wgu_d = self.inp("ffn_w_gu_%d" % li, [D, 2 * DFF])
        wd_d = self.inp("ffn_w_down_%d" % li, [DFF, D])
        g_d = self.inp("norm_ffn_bc_%d" % li, [128, D])
        NJ = DFF // 128
        with contextlib.ExitStack() as st:
            wgu = fw.sb([128, 8, 2 * DFF], BF16, "wgu", st)
            wd = fw.sb([128, NJ, D], BF16, "wd", st)
            gbc = fw.sb([128, D], F32, "gbc", st)
            hb = [fw.sb([128, D], F32, "hb%d" % i, st) for i in range(4)]
            xn = [fw.sb([128, D], BF16, "xn%d" % i, st) for i in range(2)]
            sq = fw.sb([128, D], BF16, "sq", st)
            stt_ = [[fw.sb([128, 1], F32, "st", st) for _ in range(3)] for _ in range(2)]
            xnT = [fw.sb([128, 8, 512], BF16, "xnT%d" % i, st) for i in range(2)]
            sg = [fw.sb([128, 512], F32, "sg%d" % i, st) for i in range(2)]
            actT = fw.sb([128, NJ, 512], BF16, "actT", st)
            fw.dma(fw.sp, gbc[:], g_d[:, :], w=[gbc])
            GW = 256
            NG = DFF // GW
            wg_t = [Tile(wgu.t, "wgu_g%d" % g) for g in range(NG)]
            for g in range(NG):
                for part in range(2):
                    c0_ = part * DFF + g * GW
                    for k in range(8):
                        fw.dma(fw.pool, wgu.t[:, k, c0_:c0_ + GW], wgu_d[k * 128:(k + 1) * 128, c0_:c0_ + GW], w=[wg_t[g]])
            self.load_w_bf16(wd, wd_d, DFF, D)
            src = self.src_tiles()
            TT = 512 if S >= 512 else S
            nsub = TT // 128
            cnt = 0
            for tt in range(S // TT):
                xT = xnT[tt % 2]
                for sub in range(nsub):
                    ti = tt * nsub + sub
                    self.norm_tile(src[ti], gbc, hb[sub], xn[cnt % 2], stt_[cnt % 2], sq)
                    self.transpose_to(xn[cnt % 2], xT, sub * 128, 8, [P[6], P[7]], [fw.act, fw.dve])
                    cnt += 1
                for j in range(NJ):
                    pg = P[(2 * j) % 4]
                    pu = P[(2 * j + 1) % 4]
                    for k in range(8):
                        self.mm(pg[:, 0:TT], wgu[:, k, j * 128:(j + 1) * 128], xT[:, k, 0:TT], k == 0, k == 7, [wg_t[j // 2], xT], [pg])
                    for k in range(8):
                        self.mm(pu[:, 0:TT], wgu[:, k, DFF + j * 128:DFF + (j + 1) * 128], xT[:, k, 0:TT], k == 0, k == 7, [wg_t[j // 2], xT], [pu])
                    s = sg[j % 2]
                    self.actf(s[:, 0:TT], pg[:, 0:TT], AF.Silu, [pg], [s])
                    self.tt(fw.dve, actT[:, j, 0:TT], s[:, 0:TT], pu[:, 0:TT], ALU.mult, [s, pu], [actT])
                for sub in range(nsub):
                    ti = tt * nsub + sub
                    hnew = hb[sub]
                    for half in range(2):
                        pb = P[4 + half]
                        for j in range(NJ):
                            self.mm(pb[:, :], actT[:, j, sub * 128:(sub + 1) * 128], wd[:, j, half * 512:(half + 1) * 512],
                                    j == 0, j == NJ - 1, [actT, wd], [pb])
                        self.tt(fw.dve, hnew[:, half * 512:(half + 1) * 512], hb[sub][:, half * 512:(half + 1) * 512], pb[:, :], ALU.add,
                                [hb[sub], pb], [hnew])
                    fw.dma(fw.sp, self.ht[ti].t, hnew[:], r=[hnew], w=[self.ht[ti]])
            fw.barrier()
        self.first = False

    def final_norm(self):
        fw = self.fw
        g_d = self.inp("norm_final_bc", [128, D])
        with contextlib.ExitStack() as st:
            gbc = fw.sb([128, D], F32, "gbc", st)
            hb = [fw.sb([128, D], F32, "hb%d" % i, st) for i in range(2)]
            ob = [fw.sb([128, D], F32, "ob%d" % i, st) for i in range(2)]
            sq = fw.sb([128, D], BF16, "sq", st)
            stt_ = [[fw.sb([128, 1], F32, "st", st) for _ in range(3)] for _ in range(2)]
            fw.dma(fw.sp, gbc[:], g_d[:, :], w=[gbc])
            src = self.src_tiles()
            for ti in range(self.NT):
                self.norm_tile(src[ti], gbc, hb[ti % 2], ob[ti % 2], stt_[ti % 2], sq)
                fw.dma(fw.sp, self.ot[ti].t, ob[ti % 2][:], r=[ob[ti % 2]], w=[self.ot[ti]])
            fw.barrier()

    def copy_out(self):
        fw = self.fw
        with contextlib.ExitStack() as st:
            hb = [fw.sb([128, D], F32, "hb%d" % i, st) for i in range(2)]
            src = self.src_tiles()
            for ti in range(self.NT):
                fw.dma(fw.sp, hb[ti % 2][:], src[ti].t, r=[src[ti]], w=[hb[ti % 2]])
                fw.dma(fw.sp, self.ot[ti].t, hb[ti % 2][:], r=[hb[ti % 2]], w=[self.ot[ti]])
            fw.barrier()


def default_layers():
    L = []
    for i in range(4):
        kind = ["ret", "mlstm", "rwkv"][i % 3]
        L.append((kind, i, i // 3))
        L.append(("ffn", i, 0))
    return L


def bc128(v):
    v = np.ascontiguousarray(v, dtype=np.float32).reshape(1, -1)
    return np.ascontiguousarray(np.broadcast_to(v, (128, v.shape[1])))


def host_inputs(prog, inputs, b):
    m = {}
    S = prog.S
    for name in prog.ins:
        if name == "x":
            m[name] = np.ascontiguousarray(inputs["x"][b])
        elif name == "c_ident":
            m[name] = np.eye(128, dtype=np.float32)
        elif name.startswith("ffn_w_gu_"):
            m[name] = np.ascontiguousarray(inputs["ffn_w_gu"][int(name.split("_")[-1])])
        elif name.startswith("ffn_w_down_"):
            m[name] = np.ascontiguousarray(inputs["ffn_w_down"][int(name.split("_")[-1])])
        elif name.startswith("norm_ffn_bc_"):
            m[name] = bc128(inputs["norm_ffn"][int(name.split("_")[-1])])
        elif name.startswith("norm_mix_bc_"):
            m[name] = bc128(inputs["norm_mix"][int(name.split("_")[-1])])
        elif name == "norm_final_bc":
            m[name] = bc128(inputs["norm_final"])
        else:
            m[name] = prog.host_extra(name, inputs)
        assert m[name].shape == prog.ins[name][0], (name, m[name].shape, prog.ins[name][0])
        m[name] = np.ascontiguousarray(m[name], dtype=np.float32)
    return m


_CACHE = {}


def run(inputs, S=None, layers=None, final_norm=True, ncores=8):
    x = np.asarray(inputs["x"])
    B = x.shape[0]
    S = S or x.shape[1]
    layers = layers if layers is not None else default_layers()
    key = (S, tuple(layers), final_norm)
    if key not in _CACHE:
        _CACHE[key] = Prog(S, layers, final_norm)
    prog = _CACHE[key]
    inputs = {k: np.asarray(v) for k, v in inputs.items()}
    maps = [host_inputs(prog, inputs, c % B) for c in range(min(B, ncores))]
    in_maps = [maps[c % B] for c in range(ncores)]
    res = run_bass_kernel_spmd(prog.nc, in_maps, core_ids=list(range(ncores)))
    LAST["res"] = res.results
    out = np.stack([np.asarray(res.results[b]["out"]) for b in range(B)], axis=0)
    return out.astype(np.float32)


def kernel(**inputs):
    return run(inputs)
```

```python
import contextlib
import numpy as np
import ml_dtypes
import concourse.bass as bass
import concourse.mybir as mybir
from concourse.bass_utils import run_bass_kernel_spmd

F32 = mybir.dt.float32
BF16 = mybir.dt.bfloat16
AF = mybir.ActivationFunctionType
ALU = mybir.AluOpType
AX = mybir.AxisListType

SAME_ENGINE_SYNC = True
WAW_RELAX = True
NDMASEM = 8
DEBUG = False
LAST = {}
D = 1024
DFF = 2816
RMS_EPS = 1e-6


class Tile:
    __slots__ = ("t", "lw", "rd", "name")

    def __init__(self, t, name=""):
        self.t = t
        self.lw = None
        self.rd = {}
        self.name = name

    def __getitem__(self, k):
        return self.t[k]


class Eng:
    def __init__(self, fw, name, obj, is_pe=False):
        self.fw = fw
        self.name = name
        self.o = obj
        self.sem = fw.stack.enter_context(fw.nc.semaphore("s_" + name))
        self.n = 0
        self.seen = {}
        self.is_pe = is_pe
        self.dsems = None
        self.dn = 0
        self.last = []

    def wait(self, tok):
        sem, val, en = tok
        k = id(sem)
        if self.seen.get(k, 0) >= val:
            return
        self.seen[k] = val
        self.o.wait_ge(sem, val)


class FW:
    def __init__(self):
        self.nc = bass.Bass("TRN2", target_bir_lowering=False)
        self.stack = contextlib.ExitStack()
        nc = self.nc
        self.pe = Eng(self, "pe", nc.tensor, True)
        self.act = Eng(self, "act", nc.scalar)
        self.dve = Eng(self, "dve", nc.vector)
        self.pool = Eng(self, "pool", nc.gpsimd)
        self.sp = Eng(self, "sp", nc.sync)
        self.engs = [self.pe, self.act, self.dve, self.pool, self.sp]
        for q in (self.sp, self.pool):
            q.dsems = [self.stack.enter_context(nc.semaphore("d_%s%d" % (q.name, i))) for i in range(NDMASEM)]
        self.alltok = []
        self.uid = 0

    def sb(self, shape, dtype, name=None, stack=None):
        self.uid += 1
        name = (name or "t") + "_%d" % self.uid
        t = (stack or self.stack).enter_context(self.nc.sbuf_tensor(name, list(shape), dtype))
        return Tile(t, name)

    def ps(self, shape, dtype, name=None, stack=None):
        self.uid += 1
        name = (name or "p") + "_%d" % self.uid
        t = (stack or self.stack).enter_context(self.nc.psum_tensor(name, list(shape), dtype))
        return Tile(t, name)

    def dram(self, name, shape, dtype, kind=None):
        if kind:
            t = self.nc.dram_tensor(name, list(shape), dtype, kind=kind)
        else:
            t = self.nc.dram_tensor(name, list(shape), dtype)
        return t.ap()

    def _toks(self, r, w):
        toks = []
        for b in r:
            if b.lw is not None:
                toks.append(b.lw)
        for b in w:
            if b.lw is not None:
                toks.append(b.lw)
            toks.extend(b.rd.values())
        return toks

    def _mark(self, tok, r, w, key):
        for b in w:
            b.lw = tok
            b.rd = {}
        for b in r:
            b.rd[key] = tok

    def op(self, eng, fn, r=(), w=()):
        raw = [b.lw for b in r if b.lw is not None]
        for tok in raw:
            if tok[2] == eng.name and (eng.is_pe or not SAME_ENGINE_SYNC):
                continue
            eng.wait(tok)
        for b in w:
            toks = ([b.lw] if b.lw is not None else []) + list(b.rd.values())
            for tok in toks:
                if tok[2] == eng.name and (eng.is_pe or not SAME_ENGINE_SYNC or (WAW_RELAX and eng.name in ("act", "dve"))):
                    continue
                eng.wait(tok)
        ins = fn()
        eng.n += 1
        ins.then_inc(eng.sem, 1)
        tok = (eng.sem, eng.n, eng.name)
        self._mark(tok, r, w, eng.name)
        return tok

    def dma(self, q, out, in_, r=(), w=()):
        i = q.dn % NDMASEM
        use = q.dn // NDMASEM
        sem = q.dsems[i]
        if use > 0:
            q.wait((sem, 16 * use, "dma"))
        for tok in self._toks(r, w):
            q.wait(tok)
        q.o.dma_start(out=out, in_=in_).then_inc(sem, 16)
        q.dn += 1
        tok = (sem, 16 * (use + 1), "dma")
        self._mark(tok, r, w, "dma_%s_%d" % (q.name, i))
        q.last = (q.last + [tok])[-NDMASEM:]
        return tok

    def barrier(self):
        toks = [(e.sem, e.n, e.name) for e in self.engs if e.n > 0]
        toks += self.sp.last + self.pool.last
        for e in self.engs:
            for tok in toks:
                if tok[2] == e.name:
                    continue
                e.wait(tok)

    def finish(self):
        for tok in self.sp.last + self.pool.last:
            self.sp.wait(tok)
        for e in self.engs:
            if e is not self.sp and e.n > 0:
                self.sp.wait((e.sem, e.n, e.name))
        self.barrier()
        nc = self.nc
        sems = [e.sem for e in self.engs]
        for q in (self.sp, self.pool):
            sems += list(q.dsems)
        nc.all_engine_barrier()
        nc.clear_and_free_semaphores(sems)
        nc.all_engine_barrier()


class Prog:
    def __init__(self, S, layers, final_norm=True):
        self.S = S
        self.NT = S // 128
        self.layers = layers
        self.fw = FW()
        fw = self.fw
        nc = fw.nc
        self.nc = nc
        self.ins = {}
        self.ins_ap = {}
        self.x = self.inp("x", [S, D])
        self.out = fw.dram("out", [S, D], F32, "ExternalOutput")
        self.h = fw.dram("h_scr", [S, D], F32)
        self.ht = [Tile(self.h[i * 128:(i + 1) * 128, :], "h%d" % i) for i in range(self.NT)]
        self.xt = [Tile(self.x[i * 128:(i + 1) * 128, :], "x%d" % i) for i in range(self.NT)]
        self.ot = [Tile(self.out[i * 128:(i + 1) * 128, :], "o%d" % i) for i in range(self.NT)]
        self.ident = fw.sb([128, 128], BF16, "ident")
        idd = self.inp("c_ident", [128, 128])
        fw.dma(fw.pool, self.ident[:], idd[:, :], w=[self.ident])
        self.P = [fw.ps([128, 512], F32, "bank%d" % i) for i in range(8)]
        for e in (1e-6, 64e-5):
            self.eps_tile(e)
        self.first = True
        for (kind, li, j) in layers:
            if kind == "ffn":
                self.ffn(li)
            elif kind == "ret":
                self.retention(li, j)
            elif kind == "mlstm":
                self.mlstm(li, j)
            elif kind == "rwkv":
                self.rwkv(li, j)
            fw.barrier()
        if final_norm:
            self.final_norm()
        else:
            self.copy_out()
        fw.finish()

    def inp(self, name, shape, dtype=F32):
        if name in self.ins:
            return self.ins_ap[name]
        ap = self.fw.dram(name, shape, dtype, "ExternalInput")
        self.ins[name] = (tuple(shape), dtype)
        self.ins_ap[name] = ap
        return ap

    def src_tiles(self):
        if self.first:
            return self.xt
        return self.ht

    def mm(self, out, lhsT, rhs, start, stop, r, w, sgc=False):
        nc = self.nc
        if sgc:
            return self.fw.op(self.fw.pe, lambda: nc.tensor.matmul(out, lhsT, rhs, start=start, stop=stop, skip_group_check=True), r, w)
        return self.fw.op(self.fw.pe, lambda: nc.tensor.matmul(out, lhsT, rhs, start=start, stop=stop), r, w)

    def outproj(self, y_, yT_, wout, hbuf, c, nk=16):
        fw = self.fw
        P = self.P
        self.transpose_to(y_, yT_, 0, nk, [P[6], P[7]], [fw.act, fw.dve])
        for half in range(2):
            pb = P[4 + half]
            for k in range(nk):
                self.mm(pb[:, :], yT_[:, k, :], wout[:, k, half * 512:(half + 1) * 512], k == 0, k == nk - 1, [yT_, wout], [pb])
            self.tt(fw.dve, hbuf[:, half * 512:(half + 1) * 512], hbuf[:, half * 512:(half + 1) * 512], pb[:, :], ALU.add,
                    [hbuf, pb], [hbuf])
        fw.dma(fw.sp, self.ht[c].t, hbuf[:], r=[hbuf], w=[self.ht[c]])

    def actf(self, out, in_, func, r, w, scale=1.0, bias=0.0, accum_out=None):
        nc = self.nc
        if accum_out is not None:
            return self.fw.op(self.fw.act, lambda: nc.scalar.activation(out=out, in_=in_, func=func, scale=scale, bias=bias, accum_out=accum_out), r, w)
        return self.fw.op(self.fw.act, lambda: nc.scalar.activation(out=out, in_=in_, func=func, scale=scale, bias=bias), r, w)

    def tt(self, eng, out, in0, in1, op, r, w):
        return self.fw.op(eng, lambda: eng.o.tensor_tensor(out=out, in0=in0, in1=in1, op=op), r, w)

    def ts(self, eng, out, in0, s1, s2, op0, op1, r, w):
        if s2 is None:
            return self.fw.op(eng, lambda: eng.o.tensor_scalar(out=out, in0=in0, scalar1=s1, scalar2=None, op0=op0), r, w)
        return self.fw.op(eng, lambda: eng.o.tensor_scalar(out=out, in0=in0, scalar1=s1, scalar2=s2, op0=op0, op1=op1), r, w)

    def stt(self, eng, out, in0, scalar, in1, op0, op1, r, w):
        if eng is self.fw.pool:
            eng = self.fw.dve
        return self.fw.op(eng, lambda: eng.o.scalar_tensor_tensor(out=out, in0=in0, scalar=scalar, in1=in1, op0=op0, op1=op1), r, w)

    def split2(self, src_ap, src_tiles, hi, lo, tmp, hi_ap=None, lo_ap=None, tmp_ap=None):
        fw = self.fw
        nc = self.nc
        hi_ap = hi[:] if hi_ap is None else hi_ap
        lo_ap = lo[:] if lo_ap is None else lo_ap
        tmp_ap = tmp[:] if tmp_ap is None else tmp_ap
        fw.op(fw.pool, lambda: nc.gpsimd.tensor_copy(out=hi_ap, in_=src_ap), list(src_tiles), [hi])
        self.tt(fw.dve, tmp_ap, src_ap, hi_ap, ALU.subtract, list(src_tiles) + [hi], [tmp])
        fw.op(fw.pool, lambda: nc.gpsimd.tensor_copy(out=lo_ap, in_=tmp_ap), [tmp], [lo])

    def rstd_from_ss(self, ss, std, rstd, n, eps):
        fw = self.fw
        self.actf(std[:], ss[:], AF.Sqrt, [ss], [std], scale=1.0 / n, bias=self.eps_tile(eps))
        fw.op(fw.dve, lambda: self.nc.vector.reciprocal(out=rstd[:], in_=std[:]), [std], [rstd])

    def eps_tile(self, eps):
        key = "eps_%g" % eps
        if not hasattr(self, "_eps"):
            self._eps = {}
        if key not in self._eps:
            t = self.fw.sb([128, 1], F32, "eps")
            self.fw.op(self.fw.pool, lambda: self.nc.gpsimd.memset(t[:], eps), [], [t])
            self._eps[key] = t
        return self._eps[key][:]

    def norm_tile(self, src, gbc, hbuf, xn, st, sq):
        fw = self.fw
        nc = self.nc
        ss, std, rstd = st
        fw.dma(fw.sp, hbuf[:], src.t, r=[src], w=[hbuf])
        self.actf(sq[:], hbuf[:], AF.Square, [hbuf], [sq, ss], accum_out=ss[:])
        self.rstd_from_ss(ss, std, rstd, D, RMS_EPS)
        self.stt(fw.dve, xn[:], hbuf[:], rstd[:], gbc[:], ALU.mult, ALU.mult, [hbuf, rstd, gbc], [xn])

    def transpose_to(self, xn, dstT, col0, nk, pbanks, evac_engs):
        fw = self.fw
        for g in range(0, nk, 4):
            pb = pbanks[(g // 4) % len(pbanks)]
            n = min(4, nk - g)
            for k in range(n):
                self.mm(pb[:, k * 128:(k + 1) * 128], xn[:, (g + k) * 128:(g + k + 1) * 128], self.ident[:], True, True,
                        [xn, self.ident], [pb])
            e = evac_engs[(g // 4) % len(evac_engs)]
            src = pb[:, 0:n * 128].rearrange("p (k t) -> p k t", k=n)
            dst = dstT[:, g:g + n, col0:col0 + 128]
            if e is fw.act:
                self.actf(dst, src, AF.Copy, [pb], [dstT])
            else:
                fw.op(e, lambda: e.o.tensor_copy(out=dst, in_=src), [pb], [dstT])

    def load_w_bf16(self, dst, src_ap, K, N, r=(), col0=0, ncols=None):
        fw = self.fw
        ncols = ncols or N
        kc = K // 128
        for k in range(kc):
            fw.dma(fw.pool, dst[:, k, 0:ncols], src_ap[k * 128:(k + 1) * 128, col0:col0 + ncols], w=[dst])


    def norm_all(self, gname, xnT_all, st):
        fw = self.fw
        P = self.P
        g_d = self.inp(gname, [128, D])
        gbc = fw.sb([128, D], F32, "gbc", st)
        hb = [fw.sb([128, D], F32, "hb%d" % i, st) for i in range(2)]
        xn = [fw.sb([128, D], BF16, "xn%d" % i, st) for i in range(2)]
        sq = fw.sb([128, D], BF16, "sq", st)
        stt_ = [[fw.sb([128, 1], F32, "st", st) for _ in range(3)] for _ in range(2)]
        fw.dma(fw.sp, gbc[:], g_d[:, :], w=[gbc])
        src = self.src_tiles()
        for ti in range(self.NT):
            self.norm_tile(src[ti], gbc, hb[ti % 2], xn[ti % 2], stt_[ti % 2], sq)
            self.transpose_to(xn[ti % 2], xnT_all, ti * 128, 8, [P[6], P[7]], [fw.act, fw.dve])

    def stream_fm(self, w_d, col0, xnT_all, wblks, bi, cb_fn):
        fw = self.fw
        S = self.S
        P = self.P
        wb = wblks[bi % 2]
        self.load_w_bf16(wb, w_d, D, None, col0=col0, ncols=512)
        TT = min(512, S)
        n = 0
        for tt in range(S // TT):
            for c in range(4):
                pb = P[(4 * tt + c) % 4]
                for k in range(8):
                    self.mm(pb[:, 0:TT], wb[:, k, c * 128:(c + 1) * 128], xnT_all[:, k, tt * TT:(tt + 1) * TT], k == 0, k == 7,
                            [wb, xnT_all], [pb])
                cb_fn(c, tt, pb, TT)

    def stream_tm(self, w_d, col0, xnT_all, wblks, bi, cb_fn, K=D, ncols=512):
        fw = self.fw
        P = self.P
        wb = wblks[bi % 2]
        self.load_w_bf16(wb, w_d, K, None, col0=col0, ncols=ncols)
        kc = K // 128
        for t in range(self.NT):
            pb = P[4 + (t % 2)]
            for k in range(kc):
                self.mm(pb[:, 0:ncols], xnT_all[:, k, t * 128:(t + 1) * 128], wb[:, k, 0:ncols], k == 0, k == kc - 1,
                        [wb, xnT_all], [pb])
            cb_fn(t, pb)

    def retention(self, li, j):
        fw = self.fw
        nc = self.nc
        S = self.S
        NT = self.NT
        P = self.P
        H, DK, DV, L = 4, 256, 512, 128
        w_in = self.inp("ret_w_in_%d" % j, [D, 6144])
        w_out = self.inp("ret_w_out_%d" % j, [2048, D])
        gn_d = self.inp("ret_gn_bc_%d" % j, [128, 2048])
        cos_d = self.inp("c_cosT", [128, S])
        sin_d = self.inp("c_sinT", [128, S])
        mask_d = self.inp("c_ret_maskT", [128, H * 128])
        dec_d = self.inp("c_ret_dec", [128, 8])
        if not hasattr(self, "ret_scr"):
            kd_ = "ExternalOutput" if DEBUG else None
            self.ret_scr = (fw.dram("ret_qkT", [16, 128, S], BF16, kd_), fw.dram("ret_v", [S, 2048], BF16, kd_),
                            fw.dram("ret_gs", [S, 2048], BF16, kd_))
        qk_d, v_d, gs_d = self.ret_scr
        gam = [1.0 - 2.0 ** (-5.0 - h) for h in range(H)]
        cd = [float(np.float32(g) ** L) for g in gam]
        TT = min(512, S)
        with contextlib.ExitStack() as st:
            xnT_all = fw.sb([128, 8, S], BF16, "xnT_all", st)
            cosT = fw.sb([128, S], F32, "cosT", st)
            sinT = fw.sb([128, S], F32, "sinT", st)
            fw.dma(fw.sp, cosT[:], cos_d[:, :], w=[cosT])
            fw.dma(fw.sp, sinT[:], sin_d[:, :], w=[sinT])
            wblks = [fw.sb([128, 8, 512], BF16, "wblk%d" % i, st) for i in range(2)]
            c0 = [fw.sb([128, 512], F32, "c0_%d" % i, st) for i in range(2)]
            c1 = [fw.sb([128, 512], F32, "c1_%d" % i, st) for i in range(2)]
            t1 = fw.sb([128, 512], F32, "t1", st)
            t2 = fw.sb([128, 512], F32, "t2", st)
            t3 = fw.sb([128, 512], F32, "t3", st)
            t4 = fw.sb([128, 512], F32, "t4", st)
            o1 = [fw.sb([128, 512], BF16, "o1_%d" % i, st) for i in range(2)]
            o2 = [fw.sb([128, 512], BF16, "o2_%d" % i, st) for i in range(2)]
            vst = [fw.sb([128, 512], BF16, "vst%d" % i, st) for i in range(4)]
            with contextlib.ExitStack() as st2:
                self.norm_all("norm_mix_bc_%d" % li, xnT_all, st2)
            cnt = [0]
            hold = {}
            for blk in range(4):
                sc = 1.0 if blk < 2 else DK ** -0.5

                def cb(c, tt, pb, TT_, blk=blk, sc=sc):
                    half = c % 2
                    i = cnt[0] % 2
                    dst = c0[i] if half == 0 else c1[i]
                    self.actf(dst[:, 0:TT_], pb[:, 0:TT_], AF.Copy, [pb], [dst], scale=sc)
                    if half == 0:
                        return
                    a, b = c0[i], c1[i]
                    cs = cosT[:, tt * TT_:(tt + 1) * TT_]
                    sn = sinT[:, tt * TT_:(tt + 1) * TT_]
                    self.tt(fw.dve, t1[:, 0:TT_], a[:, 0:TT_], cs, ALU.mult, [a, cosT], [t1])
                    self.tt(fw.pool, t2[:, 0:TT_], b[:, 0:TT_], sn, ALU.mult, [b, sinT], [t2])
                    self.tt(fw.pool, t3[:, 0:TT_], b[:, 0:TT_], cs, ALU.mult, [b, cosT], [t3])
                    self.tt(fw.dve, t4[:, 0:TT_], a[:, 0:TT_], sn, ALU.mult, [a, sinT], [t4])
                    self.tt(fw.dve, o1[i][:, 0:TT_], t1[:, 0:TT_], t2[:, 0:TT_], ALU.subtract, [t1, t2], [o1[i]])
                    self.tt(fw.pool, o2[i][:, 0:TT_], t3[:, 0:TT_], t4[:, 0:TT_], ALU.add, [t3, t4], [o2[i]])
                    cc = blk * 4 + c - 1
                    fw.dma(fw.sp, qk_d[cc, :, tt * TT_:(tt + 1) * TT_], o1[i][:, 0:TT_], r=[o1[i]])
                    fw.dma(fw.sp, qk_d[cc + 1, :, tt * TT_:(tt + 1) * TT_], o2[i][:, 0:TT_], r=[o2[i]])
                    cnt[0] += 1

                self.stream_fm(w_in, blk * 512, xnT_all, wblks, blk, cb)
            for blk in range(8):
                def cb(t, pb, blk=blk):
                    i = cnt[0] % 4
                    cnt[0] += 1
                    if blk < 4:
                        if t % 2 == 0:
                            self.actf(vst[i][:], pb[:, :], AF.Copy, [pb], [vst[i]])
                        else:
                            fw.op(fw.dve, lambda: nc.vector.tensor_copy(out=vst[i][:], in_=pb[:, :]), [pb], [vst[i]])
                        fw.dma(fw.sp, v_d[t * 128:(t + 1) * 128, blk * 512:(blk + 1) * 512], vst[i][:], r=[vst[i]])
                    else:
                        self.actf(vst[i][:], pb[:, :], AF.Silu, [pb], [vst[i]])
                        fw.dma(fw.sp, gs_d[t * 128:(t + 1) * 128, (blk - 4) * 512:(blk - 3) * 512], vst[i][:], r=[vst[i]])

                self.stream_tm(w_in, 2048 + blk * 512, xnT_all, wblks, blk, cb)
            fw.barrier()
        with contextlib.ExitStack() as st:
            wout = fw.sb([128, 16, D], BF16, "wout", st)
            self.load_w_bf16(wout, w_out, 2048, D)
            gnbc = fw.sb([128, 2048], F32, "gnbc", st)
            maskT = fw.sb([128, H * 128], F32, "maskT", st)
            dec = fw.sb([128, 8], F32, "dec", st)
            fw.dma(fw.sp, gnbc[:], gn_d[:, :], w=[gnbc])
            fw.dma(fw.sp, maskT[:], mask_d[:, :], w=[maskT])
            fw.dma(fw.sp, dec[:], dec_d[:, :], w=[dec])
            qk = [fw.sb([128, 16, TT], BF16, "qk%d" % i, st) for i in range(2)]
            vt = [fw.sb([128, 2048], BF16, "vt%d" % i, st) for i in range(2)]
            gt = [fw.sb([128, 2048], BF16, "gt%d" % i, st) for i in range(2)]
            ggn = [fw.sb([128, 2048], F32, "ggn%d" % i, st) for i in range(2)]
            Sst = [fw.sb([128, 2, DV], F32, "S%d" % h, st) for h in range(H)]
            Sbf = [fw.sb([128, 2, DV], BF16, "Sbf%d" % h, st) for h in range(H)]
            kd = [fw.sb([128, DK], BF16, "kd%d" % i, st) for i in range(2)]
            sT = [fw.sb([128, 128], BF16, "sT%d" % i, st) for i in range(2)]
            tmp = [fw.sb([128, DV], F32, "tmp%d" % i, st) for i in range(2)]
            ob = [fw.sb([128, DV], F32, "ob%d" % i, st) for i in range(2)]
            sq = fw.sb([128, DV], BF16, "sq", st)
            stt_ = [[fw.sb([128, 1], F32, "st", st) for _ in range(3)] for _ in range(2)]
            yb = [fw.sb([128, 2048], BF16, "yb%d" % i, st) for i in range(2)]
            yT = [fw.sb([128, 16, 128], BF16, "yT%d" % i, st) for i in range(2)]
            hb = [fw.sb([128, D], F32, "hb%d" % i, st) for i in range(2)]
            for h in range(H):
                fw.op(fw.pool, lambda: nc.gpsimd.memset(Sst[h][:], 0.0), [], [Sst[h]])
                fw.op(fw.pool, lambda: nc.gpsimd.memset(Sbf[h][:], 0.0), [], [Sbf[h]])
            src = self.src_tiles()
            cpt = TT // 128
            n = 0
            for c in range(NT):
                tt, cl = c // cpt, c % cpt
                q_ = qk[tt % 2]
                if cl == 0:
                    fw.dma(fw.sp, q_[:], qk_d[:, :, tt * TT:(tt + 1) * TT].rearrange("c p t -> p c t"), w=[q_])
                v_, g_, gg = vt[c % 2], gt[c % 2], ggn[c % 2]
                fw.dma(fw.sp, v_[:], v_d[c * 128:(c + 1) * 128, :], w=[v_])
                fw.dma(fw.sp, g_[:], gs_d[c * 128:(c + 1) * 128, :], w=[g_])
                fw.dma(fw.sp, hb[c % 2][:], src[c].t, r=[src[c]], w=[hb[c % 2]])
                self.tt(fw.pool, gg[:], g_[:], gnbc[:], ALU.mult, [g_, gnbc], [gg])
                y_ = yb[c % 2]
                cs = slice(cl * 128, (cl + 1) * 128)
                for h in range(H):
                    i = n % 2
                    n += 1
                    for half in range(2):
                        self.mm(P[0][:, half * 128:(half + 1) * 128], q_[:, 8 + 2 * h + half, cs], self.ident[:], True, True,
                                [q_, self.ident], [P[0]])
                    self.actf(kd[i][:], P[0][:, 0:DK], AF.Copy, [P[0], dec], [kd[i]], scale=dec[:, h:h + 1])
                    for half in range(2):
                        self.mm(P[1][:, 0:128], q_[:, 8 + 2 * h + half, cs], q_[:, 2 * h + half, cs], half == 0, half == 1,
                                [q_], [P[1]])
                    self.tt(fw.dve, sT[i][:], P[1][:, 0:128], maskT[:, h * 128:(h + 1) * 128], ALU.mult, [P[1], maskT], [sT[i]])
                    self.mm(P[2][:, :], sT[i][:], v_[:, h * DV:(h + 1) * DV], True, True, [sT[i], v_], [P[2]])
                    for half in range(2):
                        self.mm(P[3][:, :], q_[:, 2 * h + half, cs], Sbf[h][:, half, :], half == 0, half == 1, [q_, Sbf[h]], [P[3]])
                    self.actf(tmp[i][:], P[3][:, :], AF.Copy, [P[3], dec], [tmp[i]], scale=dec[:, 4 + h:5 + h])
                    self.tt(fw.dve, ob[i][:], tmp[i][:], P[2][:, :], ALU.add, [tmp[i], P[2]], [ob[i]])
                    ss, std, rstd = stt_[i]
                    self.actf(sq[:], ob[i][:], AF.Square, [ob[i]], [sq, ss], accum_out=ss[:])
                    self.rstd_from_ss(ss, std, rstd, DV, 1e-6)
                    self.stt(fw.dve, y_[:, h * DV:(h + 1) * DV], ob[i][:], rstd[:], gg[:, h * DV:(h + 1) * DV], ALU.mult, ALU.mult,
                             [ob[i], rstd, gg], [y_])
                    for half in range(2):
                        self.mm(P[4 + half][:, :], kd[i][:, half * 128:(half + 1) * 128], v_[:, h * DV:(h + 1) * DV], True, True,
                                [kd[i], v_], [P[4 + half]])
                    for half in range(2):
                        self.stt(fw.dve, Sst[h][:, half, :], Sst[h][:, half, :], cd[h], P[4 + half][:, :], ALU.mult, ALU.add,
                                 [P[4 + half], Sst[h]], [Sst[h]])
                    fw.op(fw.pool, lambda: nc.gpsimd.tensor_copy(out=Sbf[h][:], in_=Sst[h][:]), [Sst[h]], [Sbf[h]])
                yT_ = yT[c % 2]
                self.transpose_to(y_, yT_, 0, 16, [P[6], P[7]], [fw.act, fw.dve])
                for half in range(2):
                    pb = P[4 + half]
                    for k in range(16):
                        self.mm(pb[:, :], yT_[:, k, :], wout[:, k, half * 512:(half + 1) * 512], k == 0, k == 15, [yT_, wout], [pb])
                    self.tt(fw.dve, hb[c % 2][:, half * 512:(half + 1) * 512], hb[c % 2][:, half * 512:(half + 1) * 512], pb[:, :], ALU.add,
                            [hb[c % 2], pb], [hb[c % 2]])
                fw.dma(fw.sp, self.ht[c].t, hb[c % 2][:], r=[hb[c % 2]], w=[self.ht[c]])
            fw.barrier()
        self.first = False


    def mlstm(self, li, j):
        fw = self.fw
        nc = self.nc
        S = self.S
        NT = self.NT
        P = self.P
        H, DH = 4, 512
        NC4 = NT * 4
        w_in = self.inp("ml_w_in_%d" % j, [D, 4096])
        w_out = self.inp("ml_w_out_%d" % j, [2048, D])
        bd_d = self.inp("ml_bd_%d" % j, [128, 48 * 128])
        wg_d = self.inp("ml_wg_%d" % j, [128, 48 * 8])
        cw_d = self.inp("ml_cw_%d" % j, [128, 16 * 4])
        cb_d = self.inp("ml_cb_%d" % j, [128, 16])
        bg_d = self.inp("ml_bg_bc_%d" % j, [128, NT * 8])
        gn_d = self.inp("ml_gn_bc_%d" % j, [128, 2048])
        sk_d = self.inp("ml_skip_bc_%d" % j, [128, 2048])
        tri_d = self.inp("c_tri", [128, 128])
        idf_d = self.inp("c_identf", [128, 128])
        if not hasattr(self, "ml_scr"):
            self.ml_scr = (fw.dram("ml_qkT", [32, 128, S], BF16), fw.dram("ml_v", [S, 2048], BF16),
                           fw.dram("ml_c", [S, 2048], BF16), fw.dram("ml_zs", [S, 2048], BF16))
        qk_d, v_d, c_d, zs_d = self.ml_scr
        TT = min(512, S)
        NTT = S // TT
        with contextlib.ExitStack() as ph:
            KS = fw.sb([128, NC4], F32, "KS", ph)
            RS = fw.sb([128, NC4], F32, "RS", ph)
            SC = fw.sb([128, NC4], F32, "SC", ph)
            tri = fw.sb([128, 128], F32, "tri", ph)
            idf = fw.sb([128, 128], F32, "idf", ph)
            onesf = fw.sb([128, 128], F32, "onesf", ph)
            onesb = fw.sb([128, 1], BF16, "onesb", ph)
            one1 = fw.sb([128, 1], F32, "one1", ph)
            fw.dma(fw.sp, tri[:], tri_d[:, :], w=[tri])
            fw.dma(fw.sp, idf[:], idf_d[:, :], w=[idf])
            fw.op(fw.pool, lambda: nc.gpsimd.memset(onesf[:], 1.0), [], [onesf])
            fw.op(fw.pool, lambda: nc.gpsimd.memset(onesb[:], 1.0), [], [onesb])
            fw.op(fw.pool, lambda: nc.gpsimd.memset(one1[:], 1.0), [], [one1])
            with contextlib.ExitStack() as st:
                xnT_all = fw.sb([128, 8, S], BF16, "xnT_all", st)
                wblk = [fw.sb([128, 8, 128], BF16, "wblk%d" % i, st) for i in range(2)]
                wblk2 = [fw.sb([128, 8, 512], BF16, "wblkz%d" % i, st) for i in range(2)]
                bd = fw.sb([128, 48, 128], BF16, "bd", st)
                wg = fw.sb([128, 48, 8], BF16, "wg", st)
                cw = fw.sb([128, 16, 4], F32, "cw", st)
                cb = fw.sb([128, 16], F32, "cb", st)
                bg = fw.sb([128, NT * 8], F32, "bg", st)
                uT = fw.sb([128, S + 3], F32, "uT", st)
                acc = fw.sb([128, S], F32, "acc", st)
                cTb = fw.sb([128, S], BF16, "cTb", st)
                uTb = fw.sb([128, S], BF16, "uTb", st)
                stg = [fw.sb([128, TT], BF16, "stg%d" % i, st) for i in range(3)]
                stgv = [fw.sb([128, 4, 128], BF16, "stgv%d" % i, st) for i in range(2)]
                stgc = [fw.sb([128, 4, 128], BF16, "stgc%d" % i, st) for i in range(2)]
                vst = [fw.sb([128, 512], BF16, "vst%d" % i, st) for i in range(2)]
                G = fw.sb([128, NT, 8], F32, "G", st)
                E = fw.sb([128, NT, 4], F32, "E", st)
                SPt = fw.sb([128, NT, 4], F32, "SPt", st)
                A = fw.sb([128, NC4], F32, "A", st)
                Bn = fw.sb([128, NC4], F32, "Bn", st)
                tmpk = fw.sb([128, NC4], F32, "tmpk", st)
                mloc = fw.sb([128, 1], F32, "mloc", st)
                rows = fw.sb([1, 6, 128], F32, "rows", st)
                sph = fw.sb([128, NC4], BF16, "sph", st)
                spl = fw.sb([128, NC4], BF16, "spl", st)
                mlh = fw.sb([128, 1], BF16, "mlh", st)
                mll = fw.sb([128, 1], BF16, "mll", st)
                mlt = fw.sb([128, 1], F32, "mlt", st)
                rwh = fw.sb([1, 2, 128], BF16, "rwh", st)
                rwl = fw.sb([1, 2, 128], BF16, "rwl", st)
                rwt = fw.sb([1, 2, 128], F32, "rwt", st)
                trib = fw.sb([128, 128], BF16, "trib", st)
                onesb2 = fw.sb([128, 128], BF16, "onesb2", st)
                fw.op(fw.pool, lambda: nc.gpsimd.tensor_copy(out=trib[:], in_=tri[:]), [tri], [trib])
                fw.op(fw.pool, lambda: nc.gpsimd.memset(onesb2[:], 1.0), [], [onesb2])
                fw.dma(fw.pool, bd[:].rearrange("p a b -> p (a b)"), bd_d[:, :], w=[bd])
                fw.dma(fw.pool, wg[:].rearrange("p a b -> p (a b)"), wg_d[:, :], w=[wg])
                fw.dma(fw.sp, cw[:].rearrange("p a b -> p (a b)"), cw_d[:, :], w=[cw])
                fw.dma(fw.sp, cb[:], cb_d[:, :], w=[cb])
                fw.dma(fw.sp, bg[:], bg_d[:, :], w=[bg])
                with contextlib.ExitStack() as st2:
                    self.norm_all("norm_mix_bc_%d" % li, xnT_all, st2)
                    fw.barrier()
                fw.op(fw.dve, lambda: nc.vector.memset(P[7][:, :], 0.0), [], [P[7]])
                fw.op(fw.pool, lambda: nc.gpsimd.memset(uT[:, 0:3], 0.0), [], [uT])
                n = 0
                for cc in range(16):
                    wb = wblk[cc % 2]
                    self.load_w_bf16(wb, w_in, D, None, col0=cc * 128, ncols=128)
                    for tt in range(NTT):
                        pb = P[tt % 4]
                        for k in range(8):
                            self.mm(pb[:, 0:TT], wb[:, k, :], xnT_all[:, k, tt * TT:(tt + 1) * TT], k == 0, k == 7, [wb, xnT_all], [pb])
                        self.actf(uT[:, 3 + tt * TT:3 + (tt + 1) * TT], pb[:, 0:TT], AF.Copy, [pb], [uT])
                    e1 = fw.dve if cc % 2 == 0 else fw.pool
                    self.ts(e1, acc[:], uT[:, 0:S], cw[:, cc, 0:1], cb[:, cc:cc + 1], ALU.mult, ALU.add, [uT, cw, cb], [acc])
                    for jj in range(1, 4):
                        self.stt(e1, acc[:], uT[:, jj:jj + S], cw[:, cc, jj:jj + 1], acc[:], ALU.mult, ALU.add, [uT, cw, acc], [acc])
                    self.actf(cTb[:], acc[:], AF.Silu, [acc], [cTb])
                    e2 = fw.pool if cc % 2 == 0 else fw.dve
                    fw.op(e2, lambda: e2.o.tensor_copy(out=uTb[:], in_=uT[:, 3:3 + S]), [uT], [uTb])
                    for tt in range(NTT):
                        ts_ = slice(tt * TT, (tt + 1) * TT)
                        for wi, (srcT, gi) in enumerate(((cTb, cc), (cTb, 16 + cc), (uTb, 32 + cc))):
                            pb = P[4 + (n % 2)]
                            sg_ = stg[n % 3]
                            n += 1
                            self.mm(pb[:, 0:TT], bd[:, gi, :], srcT[:, ts_], True, True, [bd, srcT], [pb])
                            if wi == 1:
                                self.actf(sg_[:, 0:TT], pb[:, 0:TT], AF.Copy, [pb], [sg_])
                            else:
                                fw.op(fw.dve, lambda: nc.vector.tensor_copy(out=sg_[:, 0:TT], in_=pb[:, 0:TT]), [pb], [sg_])
                            if wi < 2:
                                fw.dma(fw.sp, qk_d[wi * 16 + cc, :, ts_], sg_[:, 0:TT], r=[sg_])
                            for t4 in range(TT // 128):
                                t = tt * (TT // 128) + t4
                                self.mm(P[7][:, t * 8:(t + 1) * 8], sg_[:, t4 * 128:(t4 + 1) * 128], wg[:, gi, :], False, False,
                                        [sg_, wg], [P[7]], sgc=True)
                        nq = TT // 128
                        sv, sc_ = stgv[tt % 2], stgc[tt % 2]
                        for t4 in range(nq):
                            self.mm(P[6][:, t4 * 128:(t4 + 1) * 128], uTb[:, tt * TT + t4 * 128:tt * TT + (t4 + 1) * 128], bd[:, 32 + cc, :],
                                    True, True, [uTb, bd], [P[6]])
                        self.actf(sv[:, 0:nq, :], P[6][:, 0:nq * 128].rearrange("p (a b) -> p a b", a=nq), AF.Copy, [P[6]], [sv])
                        fw.dma(fw.sp, v_d[tt * TT:(tt + 1) * TT, cc * 128:(cc + 1) * 128].rearrange("(a p) c -> p a c", p=128),
                               sv[:, 0:nq, :], r=[sv])
                        for t4 in range(nq):
                            self.mm(P[6][:, t4 * 128:(t4 + 1) * 128], cTb[:, tt * TT + t4 * 128:tt * TT + (t4 + 1) * 128], self.ident[:],
                                    True, True, [cTb, self.ident], [P[6]])
                        fw.op(fw.dve, lambda: nc.vector.tensor_copy(out=sc_[:, 0:nq, :], in_=P[6][:, 0:nq * 128].rearrange("p (a b) -> p a b", a=nq)),
                              [P[6]], [sc_])
                        fw.dma(fw.sp, c_d[tt * TT:(tt + 1) * TT, cc * 128:(cc + 1) * 128].rearrange("(a p) c -> p a c", p=128),
                               sc_[:, 0:nq, :], r=[sc_])
                cnt = [0]
                for blk in range(4):
                    def cbz(t, pb, blk=blk):
                        i = cnt[0] % 2
                        cnt[0] += 1
                        self.actf(vst[i][:], pb[:, :], AF.Silu, [pb], [vst[i]])
                        fw.dma(fw.sp, zs_d[t * 128:(t + 1) * 128, blk * 512:(blk + 1) * 512], vst[i][:], r=[vst[i]])
                    self.stream_tm(w_in, 2048 + blk * 512, xnT_all, wblk2, blk, cbz)
                G2 = G[:].rearrange("p a b -> p (a b)")
                self.tt(fw.dve, G2, P[7][:, 0:NT * 8], bg[:], ALU.add, [P[7], bg], [G])
                self.actf(E[:], G[:, :, 4:8], AF.Exp, [G], [E], scale=-1.0)
                self.actf(SPt[:], E[:], AF.Ln, [E, one1], [SPt], bias=one1[:])
                SP2 = SPt[:].rearrange("p a b -> p (a b)")
                self.split2(SP2, [SPt], sph, spl, tmpk)
                for pi, part in enumerate((sph, spl)):
                    self.mm(P[0][:, 0:NC4], trib[:], part[:], pi == 0, pi == 1, [trib, part], [P[0]])
                for pi, part in enumerate((sph, spl)):
                    self.mm(P[1][:, 0:NC4], onesb2[:], part[:], pi == 0, pi == 1, [onesb2, part], [P[1]])
                self.tt(fw.dve, A[:].rearrange("p (a b) -> p a b", b=4), G[:, :, 0:4], P[0][:, 0:NC4].rearrange("p (a b) -> p a b", b=4),
                        ALU.add, [G, P[0]], [A])
                fw.op(fw.dve, lambda: nc.vector.tensor_copy(out=Bn[:], in_=P[0][:, 0:NC4]), [P[0]], [Bn])
                self.split2(A[:], [A], sph, spl, tmpk)
                for pi, part in enumerate((sph, spl)):
                    self.mm(P[2][0:NC4, 0:128], part[:], self.ident[:], pi == 0, pi == 1, [part, self.ident], [P[2]])
                fw.op(fw.dve, lambda: nc.vector.reduce_max(out=mloc[0:NC4, :], in_=P[2][0:NC4, 0:128], axis=AX.X), [P[2]], [mloc])
                self.split2(mloc[0:NC4, :], [mloc], mlh, mll, mlt, hi_ap=mlh[0:NC4, :], lo_ap=mll[0:NC4, :], tmp_ap=mlt[0:NC4, :])
                for pi, part in enumerate((mlh, mll)):
                    self.mm(P[3][0:1, 0:NC4], part[0:NC4, 0:1], self.ident[0:NC4, 0:NC4], pi == 0, pi == 1, [part, self.ident], [P[3]])
                fw.op(fw.dve, lambda: nc.vector.tensor_copy(out=rows[0:1, 0, 0:NC4], in_=P[3][0:1, 0:NC4]), [P[3]], [rows])
                self.ts(fw.dve, rows[0:1, 1, 0:NC4], P[1][0:1, 0:NC4], -1.0, None, ALU.mult, None, [P[1]], [rows])
                for h in range(H):
                    fw.op(fw.dve, lambda: nc.vector.tensor_tensor_scan(out=rows[0:1, 2, h:NC4:4], data0=rows[0:1, 0, h:NC4:4],
                                                                      data1=rows[0:1, 1, h:NC4:4], initial=0.0, op0=ALU.max, op1=ALU.add),
                          [rows], [rows])
                self.tt(fw.dve, rows[0:1, 3, 0:NC4], rows[0:1, 2, 0:NC4], rows[0:1, 1, 0:NC4], ALU.subtract, [rows], [rows])
                fw.op(fw.dve, lambda: nc.vector.memset(rows[0:1, 4, 0:4], 0.0), [], [rows])
                if NC4 > 4:
                    fw.op(fw.dve, lambda: nc.vector.tensor_copy(out=rows[0:1, 4, 4:NC4], in_=rows[0:1, 2, 0:NC4 - 4]), [rows], [rows])
                self.tt(fw.dve, rows[0:1, 5, 0:NC4], rows[0:1, 4, 0:NC4], rows[0:1, 3, 0:NC4], ALU.subtract, [rows], [rows])
                self.split2(rows[0:1, 3, 0:NC4], [rows], rwh, rwl, rwt, hi_ap=rwh[0:1, 0, 0:NC4], lo_ap=rwl[0:1, 0, 0:NC4], tmp_ap=rwt[0:1, 0, 0:NC4])
                self.split2(rows[0:1, 5, 0:NC4], [rows], rwh, rwl, rwt, hi_ap=rwh[0:1, 1, 0:NC4], lo_ap=rwl[0:1, 1, 0:NC4], tmp_ap=rwt[0:1, 1, 0:NC4])
                for pi, part in enumerate((rwh, rwl)):
                    self.mm(P[4][:, 0:NC4], onesb2[0:1, :], part[0:1, 0, 0:NC4], pi == 0, pi == 1, [onesb2, part], [P[4]])
                for pi, part in enumerate((rwh, rwl)):
                    self.mm(P[5][:, 0:NC4], onesb2[0:1, :], part[0:1, 1, 0:NC4], pi == 0, pi == 1, [onesb2, part], [P[5]])
                self.tt(fw.dve, tmpk[:], A[:], P[4][:, 0:NC4], ALU.subtract, [A, P[4]], [tmpk])
                self.actf(KS[:], tmpk[:], AF.Exp, [tmpk], [KS])
                self.ts(fw.dve, KS[:], KS[:], float(DH ** -0.5), None, ALU.mult, None, [KS], [KS])
                self.tt(fw.dve, tmpk[:], Bn[:], P[4][:, 0:NC4], ALU.subtract, [Bn, P[4]], [tmpk])
                self.actf(RS[:], tmpk[:], AF.Exp, [tmpk], [RS])
                self.actf(SC[:], P[5][:, 0:NC4], AF.Exp, [P[5]], [SC])
                fw.barrier()
            with contextlib.ExitStack() as st:
                wout = fw.sb([128, 16, D], BF16, "wout", st)
                self.load_w_bf16(wout, w_out, 2048, D)
                gnbc = fw.sb([128, 2048], F32, "gnbc", st)
                skbc = fw.sb([128, 2048], F32, "skbc", st)
                fw.dma(fw.sp, gnbc[:], gn_d[:, :], w=[gnbc])
                fw.dma(fw.sp, skbc[:], sk_d[:, :], w=[skbc])
                qk = [fw.sb([128, 32, 128], BF16, "qk%d" % i, st) for i in range(2)]
                vt = [fw.sb([128, 2048], BF16, "vt%d" % i, st) for i in range(2)]
                ct = [fw.sb([128, 2048], BF16, "ct%d" % i, st) for i in range(2)]
                zt = [fw.sb([128, 2048], BF16, "zt%d" % i, st) for i in range(2)]
                skc = [fw.sb([128, 2048], F32, "skc%d" % i, st) for i in range(2)]
                Cst = [fw.sb([128, 4, DH], F32, "C%d" % h, st) for h in range(H)]
                Cbf = [fw.sb([128, 4, DH], BF16, "Cbf%d" % h, st) for h in range(H)]
                nst = [fw.sb([128, 4], F32, "n%d" % h, st) for h in range(H)]
                nbf = [fw.sb([128, 4], BF16, "nbf%d" % h, st) for h in range(H)]
                kd = [fw.sb([128, DH], BF16, "kd%d" % i, st) for i in range(2)]
                sT = [fw.sb([128, 128], BF16, "sT%d" % i, st) for i in range(2)]
                hh = [fw.sb([128, DH], F32, "hh%d" % i, st) for i in range(2)]
                t2 = [fw.sb([128, DH], F32, "t2%d" % i, st) for i in range(2)]
                sq = fw.sb([128, DH], BF16, "sq", st)
                sm = [[fw.sb([128, 1], F32, "sm", st) for _ in range(7)] for _ in range(2)]
                yb = [fw.sb([128, 2048], BF16, "yb%d" % i, st) for i in range(2)]
                yT = [fw.sb([128, 16, 128], BF16, "yT%d" % i, st) for i in range(2)]
                hb = [fw.sb([128, D], F32, "hb%d" % i, st) for i in range(2)]
                for h in range(H):
                    fw.op(fw.pool, lambda: nc.gpsimd.memset(Cst[h][:], 0.0), [], [Cst[h]])
                    fw.op(fw.pool, lambda: nc.gpsimd.memset(Cbf[h][:], 0.0), [], [Cbf[h]])
                    fw.op(fw.pool, lambda: nc.gpsimd.memset(nst[h][:], 0.0), [], [nst[h]])
                    fw.op(fw.pool, lambda: nc.gpsimd.memset(nbf[h][:], 0.0), [], [nbf[h]])
                src = self.src_tiles()
                n = 0
                for c in range(NT):
                    q_ = qk[c % 2]
                    fw.dma(fw.sp, q_[:], qk_d[:, :, c * 128:(c + 1) * 128].rearrange("c p t -> p c t"), w=[q_])
                    v_, c_, z_, sk_ = vt[c % 2], ct[c % 2], zt[c % 2], skc[c % 2]
                    fw.dma(fw.sp, v_[:], v_d[c * 128:(c + 1) * 128, :], w=[v_])
                    fw.dma(fw.sp, c_[:], c_d[c * 128:(c + 1) * 128, :], w=[c_])
                    fw.dma(fw.sp, z_[:], zs_d[c * 128:(c + 1) * 128, :], w=[z_])
                    fw.dma(fw.sp, hb[c % 2][:], src[c].t, r=[src[c]], w=[hb[c % 2]])
                    self.tt(fw.pool, sk_[:], c_[:], skbc[:], ALU.mult, [c_, skbc], [sk_])
                    y_ = yb[c % 2]
                    for h in range(H):
                        i = n % 2
                        n += 1
                        col = c * 4 + h
                        s1, nm, ss, std, rstd, dd, rd = sm[i]
                        for jj in range(4):
                            self.mm(P[0][:, jj * 128:(jj + 1) * 128], q_[:, 16 + 4 * h + jj, :], self.ident[:], True, True,
                                    [q_, self.ident], [P[0]])
                        self.actf(kd[i][:], P[0][:, :], AF.Copy, [P[0], KS], [kd[i]], scale=KS[:, col:col + 1])
                        for jj in range(4):
                            self.mm(P[1][:, 0:128], q_[:, 16 + 4 * h + jj, :], q_[:, 4 * h + jj, :], jj == 0, jj == 3, [q_], [P[1]])
                        self.stt(fw.dve, sT[i][:], P[1][:, 0:128], KS[:, col:col + 1], tri[:], ALU.mult, ALU.mult, [P[1], KS, tri], [sT[i]])
                        self.mm(P[2][:, :], sT[i][:], v_[:, h * DH:(h + 1) * DH], True, False, [sT[i], v_], [P[2]])
                        for jj in range(4):
                            self.mm(P[2][:, :], q_[:, 4 * h + jj, :], Cbf[h][:, jj, :], False, jj == 3, [q_, Cbf[h]], [P[2]])
                        self.mm(P[3][:, 0:1], sT[i][:], onesb[:], True, False, [sT[i], onesb], [P[3]])
                        for jj in range(4):
                            self.mm(P[3][:, 0:1], q_[:, 4 * h + jj, :], nbf[h][:, jj:jj + 1], False, jj == 3, [q_, nbf[h]], [P[3]])
                        self.actf(dd[:], P[3][:, 0:1], AF.Abs, [P[3]], [dd])
                        self.tt(fw.dve, dd[:], dd[:], RS[:, col:col + 1], ALU.max, [dd, RS], [dd])
                        fw.op(fw.dve, lambda: nc.vector.reciprocal(out=rd[:], in_=dd[:]), [dd], [rd])
                        self.actf(hh[i][:], P[2][:, :], AF.Copy, [P[2], rd], [hh[i], s1], scale=rd[:], accum_out=s1[:])
                        self.ts(fw.dve, nm[:], s1[:], -1.0 / DH, None, ALU.mult, None, [s1], [nm])
                        self.actf(sq[:], hh[i][:], AF.Square, [hh[i], nm], [sq, ss], bias=nm[:], accum_out=ss[:])
                        self.rstd_from_ss(ss, std, rstd, DH, 1e-6)
                        hs = slice(h * DH, (h + 1) * DH)
                        self.stt(fw.pool, t2[i][:], hh[i][:], nm[:], gnbc[:, hs], ALU.add, ALU.mult, [hh[i], nm, gnbc], [t2[i]])
                        self.stt(fw.dve, t2[i][:], t2[i][:], rstd[:], sk_[:, hs], ALU.mult, ALU.add, [t2[i], rstd, sk_], [t2[i]])
                        self.tt(fw.pool, y_[:, hs], t2[i][:], z_[:, hs], ALU.mult, [t2[i], z_], [y_])
                        for jj in range(4):
                            self.mm(P[3][:, 1 + jj:2 + jj], kd[i][:, jj * 128:(jj + 1) * 128], onesb[:], True, True, [kd[i], onesb], [P[3]])
                        self.stt(fw.dve, nst[h][:], nst[h][:], SC[:, col:col + 1], P[3][:, 1:5], ALU.mult, ALU.add, [nst[h], SC, P[3]], [nst[h]])
                        for jj in range(4):
                            pb = P[4 + (jj % 2)]
                            self.mm(pb[:, :], kd[i][:, jj * 128:(jj + 1) * 128], v_[:, hs], True, True, [kd[i], v_], [pb])
                            self.stt(fw.dve, Cst[h][:, jj, :], Cst[h][:, jj, :], SC[:, col:col + 1], pb[:, :], ALU.mult, ALU.add,
                                     [Cst[h], SC, pb], [Cst[h]])
                        if c + 1 < NT:
                            ncol = (c + 1) * 4 + h
                            self.actf(Cbf[h][:], Cst[h][:], AF.Copy, [Cst[h], SC], [Cbf[h]], scale=SC[:, ncol:ncol + 1])
                            self.ts(fw.pool, nbf[h][:], nst[h][:], SC[:, ncol:ncol + 1], None, ALU.mult, None, [nst[h], SC], [nbf[h]])
                    self.outproj(y_, yT[c % 2], wout, hb[c % 2], c)
                fw.barrier()
        self.first = False


    def rwkv(self, li, j):
        fw = self.fw
        nc = self.nc
        S = self.S
        NT = self.NT
        P = self.P
        H, DH = 16, 64
        wrkv_d = self.inp("rw_w_rkv_%d" % j, [3 * D, D])
        wout_d = self.inp("rw_w_out_%d" % j, [D, D])
        la_d = self.inp("rw_lora_a_%d" % j, [D, 288])
        lbw_d = self.inp("rw_w_lora_b_%d" % j, [64, D])
        lba_d = self.inp("rw_a_lora_b_%d" % j, [64, D])
        lbg_d = self.inp("rw_g_lora_b_%d" % j, [160, D])
        mu_d = self.inp("rw_muT_%d" % j, [128, 48])
        vec_d = self.inp("rw_vec_bc_%d" % j, [128, 7 * D])
        msk_d = self.inp("c_rw_masks", [128, 4 * 512])
        mx_d = self.inp("c_rw_mx", [128, 3 * 128])
        if not hasattr(self, "rw_scr"):
            self.rw_scr = (fw.dram("rw_rkv", [3, S, D], BF16), fw.dram("rw_logw", [S, D], F32),
                           fw.dram("rw_ag", [2, S, D], BF16))
        rkv_d, lw_d, ag_d = self.rw_scr
        with contextlib.ExitStack() as st:
            wrkv = fw.sb([128, 24, D], BF16, "wrkv", st)
            lora_a = fw.sb([128, 8, 288], BF16, "lora_a", st)
            lbw = fw.sb([64, D], BF16, "lbw", st)
            lba = fw.sb([64, D], BF16, "lba", st)
            lbg0 = fw.sb([128, D], BF16, "lbg0", st)
            lbg1 = fw.sb([32, D], BF16, "lbg1", st)
            mu = fw.sb([128, 8, 6], F32, "mu", st)
            vec = fw.sb([128, 2, D], F32, "vec", st)
            gbc = fw.sb([128, D], F32, "gbc", st)
            hb = [fw.sb([128, D], F32, "hb%d" % i, st) for i in range(2)]
            xn = [fw.sb([128, D], BF16, "xn%d" % i, st) for i in range(2)]
            sq = fw.sb([128, D], BF16, "sq", st)
            stt_ = [[fw.sb([128, 1], F32, "st", st) for _ in range(3)] for _ in range(2)]
            xnT = [fw.sb([128, 8, 129], BF16, "xnT%d" % i, st) for i in range(2)]
            dd = fw.sb([128, 8, 128], BF16, "dd", st)
            mix = [fw.sb([128, 8, 128], BF16, "mix%d" % i, st) for i in range(3)]
            l1w = fw.sb([64, 128], BF16, "l1w", st)
            l1a = fw.sb([64, 128], BF16, "l1a", st)
            l1g0 = fw.sb([128, 128], BF16, "l1g0", st)
            l1g1 = fw.sb([32, 128], BF16, "l1g1", st)
            ob = [fw.sb([128, D], BF16, "ob%d" % i, st) for i in range(4)]
            of = [fw.sb([128, D], F32, "of%d" % i, st) for i in range(2)]
            g_d = self.inp("norm_mix_bc_%d" % li, [128, D])
            fw.dma(fw.sp, gbc[:], g_d[:, :], w=[gbc])
            fw.dma(fw.sp, mu[:].rearrange("p a b -> p (a b)"), mu_d[:, :], w=[mu])
            fw.dma(fw.sp, vec[:].rearrange("p a b -> p (a b)"), vec_d[:, 0:2 * D], w=[vec])
            self.load_w_bf16(wrkv, wrkv_d, 3 * D, D)
            self.load_w_bf16(lora_a, la_d, D, 288)
            fw.dma(fw.pool, lbw[:], lbw_d[:, :], w=[lbw])
            fw.dma(fw.pool, lba[:], lba_d[:, :], w=[lba])
            fw.dma(fw.pool, lbg0[:], lbg_d[0:128, :], w=[lbg0])
            fw.dma(fw.pool, lbg1[:], lbg_d[128:160, :], w=[lbg1])
            fw.op(fw.pool, lambda: nc.gpsimd.memset(xnT[1][:, :, 128:129], 0.0), [], [xnT[1]])
            src = self.src_tiles()
            nob = 0
            for c in range(NT):
                xT = xnT[c % 2]
                xTp = xnT[(c + 1) % 2]
                self.norm_tile(src[c], gbc, hb[c % 2], xn[c % 2], stt_[c % 2], sq)
                fw.op(fw.pool, lambda: nc.gpsimd.tensor_copy(out=xT[:, :, 0:1], in_=xTp[:, :, 128:129]), [xTp], [xT])
                self.transpose_to(xn[c % 2], xT, 1, 8, [P[6], P[7]], [fw.act, fw.dve])
                self.tt(fw.pool, dd[:], xT[:, :, 0:128], xT[:, :, 1:129], ALU.subtract, [xT], [dd])
                for i in range(6):
                    mx = mix[i % 3]
                    for k in range(8):
                        self.stt(fw.dve, mx[:, k, :], dd[:, k, :], mu[:, k, i:i + 1], xT[:, k, 1:129], ALU.mult, ALU.add, [dd, mu, xT], [mx])
                    if i < 3:
                        o_ = ob[nob % 4]
                        nob += 1
                        for half in range(2):
                            pb = P[(2 * i + half) % 4]
                            for k in range(8):
                                self.mm(pb[:, :], mx[:, k, :], wrkv[:, i * 8 + k, half * 512:(half + 1) * 512], k == 0, k == 7, [mx, wrkv], [pb])
                            if half == 0:
                                self.actf(o_[:, 0:512], pb[:, :], AF.Copy, [pb], [o_])
                            else:
                                fw.op(fw.dve, lambda: nc.vector.tensor_copy(out=o_[:, 512:1024], in_=pb[:, :]), [pb], [o_])
                        fw.dma(fw.sp, rkv_d[i, c * 128:(c + 1) * 128, :], o_[:], r=[o_])
                    elif i == 3:
                        for k in range(8):
                            self.mm(P[4][0:64, 0:128], lora_a[:, k, 0:64], mx[:, k, :], k == 0, k == 7, [lora_a, mx], [P[4]])
                        self.actf(l1w[:], P[4][0:64, 0:128], AF.Tanh, [P[4]], [l1w])
                        o_ = of[c % 2]
                        for half in range(2):
                            pb = P[half]
                            self.mm(pb[:, :], l1w[:], lbw[:, half * 512:(half + 1) * 512], True, True, [l1w, lbw], [pb])
                            self.tt(fw.dve, o_[:, half * 512:(half + 1) * 512], pb[:, :], vec[:, 0, half * 512:(half + 1) * 512], ALU.add, [pb, vec], [o_])
                        self.actf(o_[:], o_[:], AF.Sigmoid, [o_], [o_])
                        self.ts(fw.pool, o_[:], o_[:], -0.6065306597126334, None, ALU.mult, None, [o_], [o_])
                        fw.dma(fw.sp, lw_d[c * 128:(c + 1) * 128, :], o_[:], r=[o_])
                    elif i == 4:
                        for k in range(8):
                            self.mm(P[4][0:64, 128:256], lora_a[:, k, 64:128], mx[:, k, :], k == 0, k == 7, [lora_a, mx], [P[4]])
                        self.actf(l1a[:], P[4][0:64, 128:256], AF.Copy, [P[4]], [l1a])
                        o_ = ob[nob % 4]
                        nob += 1
                        for half in range(2):
                            pb = P[2 + half]
                            self.mm(pb[:, :], l1a[:], lba[:, half * 512:(half + 1) * 512], True, True, [l1a, lba], [pb])
                            self.tt(fw.dve, sq[:, half * 512:(half + 1) * 512], pb[:, :], vec[:, 1, half * 512:(half + 1) * 512], ALU.add, [pb, vec], [sq])
                        self.actf(o_[:], sq[:], AF.Sigmoid, [sq], [o_])
                        fw.dma(fw.sp, ag_d[0, c * 128:(c + 1) * 128, :], o_[:], r=[o_])
                    else:
                        for k in range(8):
                            self.mm(P[5][:, 0:128], lora_a[:, k, 128:256], mx[:, k, :], k == 0, k == 7, [lora_a, mx], [P[5]])
                        for k in range(8):
                            self.mm(P[5][0:32, 128:256], lora_a[:, k, 256:288], mx[:, k, :], k == 0, k == 7, [lora_a, mx], [P[5]])
                        self.actf(l1g0[:], P[5][:, 0:128], AF.Sigmoid, [P[5]], [l1g0])
                        self.actf(l1g1[:], P[5][0:32, 128:256], AF.Sigmoid, [P[5]], [l1g1])
                        o_ = ob[nob % 4]
                        nob += 1
                        for half in range(2):
                            pb = P[half]
                            self.mm(pb[:, :], l1g0[:], lbg0[:, half * 512:(half + 1) * 512], True, False, [l1g0, lbg0], [pb])
                            self.mm(pb[:, :], l1g1[:], lbg1[:, half * 512:(half + 1) * 512], False, True, [l1g1, lbg1], [pb])
                            if half == 0:
                                self.actf(o_[:, 0:512], pb[:, :], AF.Copy, [pb], [o_])
                            else:
                                fw.op(fw.dve, lambda: nc.vector.tensor_copy(out=o_[:, 512:1024], in_=pb[:, :]), [pb], [o_])
                        fw.dma(fw.sp, ag_d[1, c * 128:(c + 1) * 128, :], o_[:], r=[o_])
            fw.barrier()
        with contextlib.ExitStack() as st:
            wout = fw.sb([128, 8, D], BF16, "wout", st)
            self.load_w_bf16(wout, wout_d, D, D)
            vec = fw.sb([128, 5, D], F32, "vec", st)
            fw.dma(fw.sp, vec[:].rearrange("p a b -> p (a b)"), vec_d[:, 2 * D:7 * D], w=[vec])
            msk = fw.sb([128, 4, 512], F32, "msk", st)
            fw.dma(fw.sp, msk[:].rearrange("p a b -> p (a b)"), msk_d[:, :], w=[msk])
            mxb = fw.sb([128, 4, 128], BF16, "mxb", st)
            fw.dma(fw.pool, mxb[:, 0:3, :].rearrange("p a b -> p (a b)"), mx_d[:, :], w=[mxb])
            fw.op(fw.pool, lambda: nc.gpsimd.memset(mxb[:, 3, :], 1.0), [], [mxb])
            lwh = fw.sb([128, D], BF16, "lwh", st)
            lwl = fw.sb([128, D], BF16, "lwl", st)
            rb = [fw.sb([128, D], BF16, "rb%d" % i, st) for i in range(2)]
            kb = [fw.sb([128, D], BF16, "kb%d" % i, st) for i in range(2)]
            vb = [fw.sb([128, D], BF16, "vb%d" % i, st) for i in range(2)]
            al = [fw.sb([128, D], BF16, "al%d" % i, st) for i in range(2)]
            gb = [fw.sb([128, D], BF16, "gb%d" % i, st) for i in range(2)]
            lw = [fw.sb([128, D], F32, "lw%d" % i, st) for i in range(2)]
            hb = [fw.sb([128, D], F32, "hb%d" % i, st) for i in range(2)]
            T = [fw.sb([128, D], F32, "T%d" % i, st) for i in range(7)]
            Xtm = [fw.sb([128, D], BF16, "Xtm%d" % i, st) for i in range(4)]
            kh = fw.sb([128, D], BF16, "kh", st)
            bh = fw.sb([128, D], BF16, "bh", st)
            XT = [fw.sb([64, 16, 128], BF16, "XT%d" % i, st) for i in range(4)]
            Agg = [[fw.sb([128, 4, 128], BF16, "Ag%d" % i, st) for i in range(2)] for _ in range(4)]
            Aak = fw.sb([128, 16, 128], BF16, "Aak", st)
            RBt = fw.sb([128, 16, 128], BF16, "RBt", st)
            RKt = fw.sb([128, 16, 128], BF16, "RKt", st)
            Wall = fw.sb([128, 16, 128], BF16, "Wall", st)
            Xsg = [[fw.sb([128, 4, 128], BF16, "Xs%d" % i, st) for i in range(2)] for _ in range(4)]
            XTsg = [[fw.sb([128, 4, 128], BF16, "XTs%d" % i, st) for i in range(2)] for _ in range(4)]
            Wsg = [[fw.sb([128, 4, 128], BF16, "Ws%d" % i, st) for i in range(2)] for _ in range(4)]
            Mst = fw.sb([64, 16, 64], F32, "Mst", st)
            Mbf = fw.sb([64, 16, 64], BF16, "Mbf", st)
            pmpl = fw.sb([64, 32], F32, "pmpl", st)
            RHSb = fw.sb([128, D], BF16, "RHSb", st)
            Ub = fw.sb([128, D], BF16, "Ub", st)
            s16 = [fw.sb([128, 16], F32, "s16_%d" % i, st) for i in range(6)]
            yb = [fw.sb([128, D], BF16, "yb%d" % i, st) for i in range(2)]
            yT = [fw.sb([128, 8, 128], BF16, "yT%d" % i, st) for i in range(2)]
            fw.op(fw.pool, lambda: nc.gpsimd.memset(Mst[:], 0.0), [], [Mst])
            src = self.src_tiles()

            def v3(t):
                return t.rearrange("p (h d) -> p h d", h=16)

            def bc16(t):
                return t[:].unsqueeze(2).to_broadcast([128, 16, 64])

            for c in range(NT):
                i2 = c % 2
                rows = slice(c * 128, (c + 1) * 128)
                r_, k_, v_, a_, g_, lw_ = rb[i2], kb[i2], vb[i2], al[i2], gb[i2], lw[i2]
                fw.dma(fw.sp, r_[:], rkv_d[0, rows, :], w=[r_])
                fw.dma(fw.sp, k_[:], rkv_d[1, rows, :], w=[k_])
                fw.dma(fw.sp, v_[:], rkv_d[2, rows, :], w=[v_])
                fw.dma(fw.sp, lw_[:], lw_d[rows, :], w=[lw_])
                fw.dma(fw.sp, a_[:], ag_d[0, rows, :], w=[a_])
                fw.dma(fw.sp, g_[:], ag_d[1, rows, :], w=[g_])
                fw.dma(fw.sp, hb[i2][:], src[c].t, r=[src[c]], w=[hb[i2]])
                T1, T2, T3, T4, T5, T6, T7 = T
                ss16, sd16, rn16, bon16, m16, rs16 = s16
                self.tt(fw.pool, T5[:], k_[:], vec[:, 0, :], ALU.mult, [k_, vec], [T5])
                self.tt(fw.pool, T6[:], T5[:], T5[:], ALU.mult, [T5], [T6])
                fw.op(fw.dve, lambda: nc.vector.tensor_reduce(out=ss16[:], in_=v3(T6[:]), axis=AX.X, op=ALU.add), [T6], [ss16])
                self.ts(fw.dve, ss16[:], ss16[:], 1e-24, None, ALU.max, None, [ss16], [ss16])
                self.actf(sd16[:], ss16[:], AF.Sqrt, [ss16], [sd16])
                fw.op(fw.dve, lambda: nc.vector.reciprocal(out=rn16[:], in_=sd16[:]), [sd16], [rn16])
                self.tt(fw.dve, v3(T5[:]), v3(T5[:]), bc16(rn16), ALU.mult, [T5, rn16], [T5])
                self.stt(fw.dve, T6[:], a_[:], -1.0, vec[:, 1, :], ALU.add, ALU.mult, [a_, vec], [T6])
                self.stt(fw.dve, T2[:], T6[:], 1.0, k_[:], ALU.add, ALU.mult, [T6, k_], [T2])
                self.tt(fw.pool, T6[:], r_[:], T2[:], ALU.mult, [r_, T2], [T6])
                self.tt(fw.pool, T6[:], T6[:], vec[:, 2, :], ALU.mult, [T6, vec], [T6])
                fw.op(fw.dve, lambda: nc.vector.tensor_reduce(out=bon16[:], in_=v3(T6[:]), axis=AX.X, op=ALU.add), [T6], [bon16])
                self.tt(fw.pool, T6[:], T5[:], a_[:], ALU.mult, [T5, a_], [T6])
                self.split2(lw_[:], [lw_], lwh, lwl, T7)
                for half in range(2):
                    hs = slice(half * 512, (half + 1) * 512)
                    for pi, part in enumerate((lwh, lwl)):
                        self.mm(P[half][:, :], mxb[:, 0, :], part[:, hs], pi == 0, pi == 1, [mxb, part], [P[half]])
                    for pi, part in enumerate((lwh, lwl)):
                        self.mm(P[2 + half][:, :], mxb[:, 1, :], part[:, hs], pi == 0, pi == 1, [mxb, part], [P[2 + half]])
                for h in range(H):
                    for pi, part in enumerate((lwh, lwl)):
                        self.mm(P[4][0:64, h:h + 1], part[:, h * 64:(h + 1) * 64], mxb[:, 2, 0:1], pi == 0, pi == 1, [part, mxb], [P[4]])
                for h in range(H):
                    for pi, part in enumerate((lwh, lwl)):
                        self.mm(P[4][0:64, 16 + h:17 + h], part[:, h * 64:(h + 1) * 64], mxb[:, 3, 0:1], pi == 0, pi == 1, [part, mxb], [P[4]])
                self.actf(pmpl[:], P[4][0:64, 0:32], AF.Exp, [P[4]], [pmpl])
                for half in range(2):
                    hs = slice(half * 512, (half + 1) * 512)
                    self.actf(T4[:, hs], P[half][:, :], AF.Exp, [P[half]], [T4])
                    self.actf(T1[:, hs], P[half][:, :], AF.Exp, [P[half]], [T1], scale=-1.0)
                    self.tt(fw.dve, T3[:, hs], P[half][:, :], lw_[:, hs], ALU.subtract, [P[half], lw_], [T3])
                    self.actf(T7[:, hs], P[2 + half][:, :], AF.Exp, [P[2 + half]], [T7])
                self.actf(T3[:], T3[:], AF.Exp, [T3], [T3])
                self.tt(fw.pool, Xtm[0][:], r_[:], T4[:], ALU.mult, [r_, T4], [Xtm[0]])
                self.tt(fw.dve, Xtm[1][:], T2[:], T1[:], ALU.mult, [T2, T1], [Xtm[1]])
                self.tt(fw.pool, Xtm[2][:], T6[:], T1[:], ALU.mult, [T6, T1], [Xtm[2]])
                self.stt(fw.dve, Xtm[3][:], T5[:], -1.0, T3[:], ALU.mult, ALU.mult, [T5, T3], [Xtm[3]])
                self.tt(fw.dve, kh[:], T2[:], T7[:], ALU.mult, [T2, T7], [kh])
                self.tt(fw.pool, bh[:], T6[:], T7[:], ALU.mult, [T6, T7], [bh])
                self.tt(fw.dve, Mbf[:], Mst[:], pmpl[:, 0:16].unsqueeze(2).to_broadcast([64, 16, 64]), ALU.mult, [Mst, pmpl], [Mbf])
                n = 0
                for q in range(4):
                    for g in range(4):
                        pb = P[4 + (n % 4)]
                        for jj in range(4):
                            h = g * 4 + jj
                            self.mm(pb[0:64, jj * 128:(jj + 1) * 128], Xtm[q][:, h * 64:(h + 1) * 64], self.ident[:], True, True,
                                    [Xtm[q], self.ident], [pb])
                        dst = XT[q][:, g * 4:(g + 1) * 4, :]
                        srcp = pb[0:64, :].rearrange("p (a b) -> p a b", a=4)
                        if n % 2 == 0:
                            self.actf(dst, srcp, AF.Copy, [pb], [XT[q]])
                        else:
                            fw.op(fw.dve, lambda: nc.vector.tensor_copy(out=dst, in_=srcp), [pb], [XT[q]])
                        n += 1
                rT, kT, bT, aT = XT
                cur = []
                for g in range(4):
                    Ag = Agg[g]
                    specs = ((aT, bT, 0, Ag[0], None), (bT, aT, 1, Ag[1], None), (kT, aT, 1, Aak, g), (bT, rT, 2, RBt, g), (kT, rT, 2, RKt, g))
                    for si, (lt, rt, mi, dstT, gg) in enumerate(specs):
                        pb = P[(g * 5 + si) % 8]
                        for jj in range(4):
                            h = g * 4 + jj
                            self.mm(pb[:, jj * 128:(jj + 1) * 128], lt[:, h, :], rt[:, h, :], True, True, [lt, rt], [pb])
                        dst = dstT[:].rearrange("p a b -> p (a b)") if gg is None else dstT[:, g * 4:(g + 1) * 4, :].rearrange("p a b -> p (a b)")
                        if si % 2 == 0:
                            self.tt(fw.dve, dst, pb[:, :], msk[:, mi, :], ALU.mult, [pb, msk], [dstT])
                        else:
                            self.tt(fw.dve, dst, pb[:, :], msk[:, mi, :], ALU.mult, [pb, msk], [dstT])
                    W = Wsg[g][0]
                    self.tt(fw.pool, W[:].rearrange("p a b -> p (a b)"), Ag[1][:].rearrange("p a b -> p (a b)"), msk[:, 3, :], ALU.add, [Ag[1], msk], [W])
                    cur.append([Ag[0], Ag[1], W])
                for lvl in range(1, 7):
                    for g in range(4):
                        X, XTt, W = cur[g]
                        Xn, XTn, Wn = Xsg[g][lvl % 2], XTsg[g][lvl % 2], Wsg[g][lvl % 2]
                        pX, pXT = P[2 * g], P[2 * g + 1]
                        for jj in range(4):
                            self.mm(pX[:, jj * 128:(jj + 1) * 128], XTt[:, jj, :], X[:, jj, :], True, True, [XTt, X], [pX])
                        self.actf(Xn[:].rearrange("p a b -> p (a b)"), pX[:, :], AF.Copy, [pX], [Xn])
                        if lvl < 6:
                            for jj in range(4):
                                self.mm(pXT[:, jj * 128:(jj + 1) * 128], X[:, jj, :], XTt[:, jj, :], True, True, [XTt, X], [pXT])
                            if g % 2 == 0:
                                self.actf(XTn[:].rearrange("p a b -> p (a b)"), pXT[:, :], AF.Copy, [pXT], [XTn])
                            else:
                                fw.op(fw.dve, lambda: nc.vector.tensor_copy(out=XTn[:].rearrange("p a b -> p (a b)"), in_=pXT[:, :]), [pXT], [XTn])
                        cur[g] = [Xn, XTn, W]
                    for g in range(4):
                        Xn, XTn, W = cur[g]
                        Wn = Wsg[g][lvl % 2]
                        pW = P[2 * g]
                        for jj in range(4):
                            self.mm(pW[:, jj * 128:(jj + 1) * 128], Xn[:, jj, :], W[:, jj, :], True, True, [Xn, W], [pW])
                        if lvl < 6:
                            self.tt(fw.dve, Wn[:].rearrange("p a b -> p (a b)"), W[:].rearrange("p a b -> p (a b)"), pW[:, :], ALU.add, [W, pW], [Wn])
                        else:
                            self.tt(fw.dve, Wall[:, g * 4:(g + 1) * 4, :].rearrange("p a b -> p (a b)"), W[:].rearrange("p a b -> p (a b)"), pW[:, :], ALU.add,
                                    [W, pW], [Wall])
                        cur[g] = [Xn, XTn, Wn]
                for h in range(H):
                    pb = P[h // 8]
                    cs = slice((h % 8) * 64, (h % 8) * 64 + 64)
                    hs = slice(h * 64, (h + 1) * 64)
                    self.mm(pb[:, cs], aT[:, h, :], Mbf[:, h, :], True, False, [aT, Mbf], [pb])
                    self.mm(pb[:, cs], Aak[:, h, :], v_[:, hs], False, True, [Aak, v_], [pb])
                self.actf(RHSb[:, 0:512], P[0][:, :], AF.Copy, [P[0]], [RHSb])
                fw.op(fw.dve, lambda: nc.vector.tensor_copy(out=RHSb[:, 512:1024], in_=P[1][:, :]), [P[1]], [RHSb])
                for h in range(H):
                    pb = P[2 + h // 8]
                    cs = slice((h % 8) * 64, (h % 8) * 64 + 64)
                    hs = slice(h * 64, (h + 1) * 64)
                    self.mm(pb[:, cs], Wall[:, h, :], RHSb[:, hs], True, True, [Wall, RHSb], [pb])
                self.actf(Ub[:, 0:512], P[2][:, :], AF.Copy, [P[2]], [Ub])
                fw.op(fw.dve, lambda: nc.vector.tensor_copy(out=Ub[:, 512:1024], in_=P[3][:, :]), [P[3]], [Ub])
                for h in range(H):
                    pb = P[6 + h // 8]
                    cs = slice((h % 8) * 64, (h % 8) * 64 + 64)
                    hs = slice(h * 64, (h + 1) * 64)
                    self.mm(pb[0:64, cs], bh[:, hs], Ub[:, hs], True, False, [bh, Ub], [pb])
                    self.mm(pb[0:64, cs], kh[:, hs], v_[:, hs], False, True, [kh, v_], [pb])
                for h in range(H):
                    pb = P[4 + h // 8]
                    cs = slice((h % 8) * 64, (h % 8) * 64 + 64)
                    hs = slice(h * 64, (h + 1) * 64)
                    self.mm(pb[:, cs], rT[:, h, :], Mbf[:, h, :], True, False, [rT, Mbf], [pb])
                    self.mm(pb[:, cs], RBt[:, h, :], Ub[:, hs], False, False, [RBt, Ub], [pb])
                    self.mm(pb[:, cs], RKt[:, h, :], v_[:, hs], False, True, [RKt, v_], [pb])
                self.tt(fw.dve, Mst[:], Mst[:], pmpl[:, 16:32].unsqueeze(2).to_broadcast([64, 16, 64]), ALU.mult, [Mst, pmpl], [Mst])
                M2 = Mst[:].rearrange("p a b -> p (a b)")
                self.tt(fw.dve, M2[:, 0:512], M2[:, 0:512], P[6][0:64, :], ALU.add, [Mst, P[6]], [Mst])
                self.tt(fw.dve, M2[:, 512:1024], M2[:, 512:1024], P[7][0:64, :], ALU.add, [Mst, P[7]], [Mst])
                self.actf(T1[:, 0:512], P[4][:, :], AF.Copy, [P[4]], [T1])
                fw.op(fw.dve, lambda: nc.vector.tensor_copy(out=T1[:, 512:1024], in_=P[5][:, :]), [P[5]], [T1])
                fw.op(fw.dve, lambda: nc.vector.tensor_reduce(out=m16[:], in_=v3(T1[:]), axis=AX.X, op=ALU.add), [T1], [m16])
                self.ts(fw.dve, m16[:], m16[:], -1.0 / DH, None, ALU.mult, None, [m16], [m16])
                self.tt(fw.pool, v3(T1[:]), v3(T1[:]), bc16(m16), ALU.add, [T1, m16], [T1])
                self.tt(fw.pool, T4[:], T1[:], T1[:], ALU.mult, [T1], [T4])
                fw.op(fw.dve, lambda: nc.vector.tensor_reduce(out=ss16[:], in_=v3(T4[:]), axis=AX.X, op=ALU.add), [T4], [ss16])
                self.actf(sd16[:], ss16[:], AF.Sqrt, [ss16], [sd16], scale=1.0 / DH, bias=self.eps_tile(64e-5)[0:128, :])
                fw.op(fw.dve, lambda: nc.vector.reciprocal(out=rs16[:], in_=sd16[:]), [sd16], [rs16])
                self.tt(fw.dve, v3(T1[:]), v3(T1[:]), bc16(rs16), ALU.mult, [T1, rs16], [T1])
                self.tt(fw.pool, T1[:], T1[:], vec[:, 3, :], ALU.mult, [T1, vec], [T1])
                self.tt(fw.pool, T1[:], T1[:], vec[:, 4, :], ALU.add, [T1, vec], [T1])
                self.tt(fw.dve, v3(T4[:]), v3(v_[:]), bc16(bon16), ALU.mult, [v_, bon16], [T4])
                self.tt(fw.pool, T1[:], T1[:], T4[:], ALU.add, [T1, T4], [T1])
                self.tt(fw.dve, yb[i2][:], T1[:], g_[:], ALU.mult, [T1, g_], [yb[i2]])
                self.outproj(yb[i2], yT[i2], wout, hb[i2], c, nk=8)
            fw.barrier()
        self.first = False

    def host_extra(self, name, inputs):
        S = self.S
        f32 = np.float32
        if name.startswith("ret_w_in_"):
            return inputs["ret_w_in"][int(name[-1])]
        if name.startswith("ret_w_out_"):
            return inputs["ret_w_out"][int(name[-1])]
        if name.startswith("ret_gn_bc_"):
            return bc128(inputs["ret_gn"][int(name[-1])])
        if name in ("c_cosT", "c_sinT"):
            pos = np.arange(S, dtype=f32)
            inv = (f32(1.0) / (f32(10000.0) ** np.linspace(0.0, 1.0, 128, dtype=f32))).astype(f32)
            ang = (pos[:, None] * inv[None, :]).astype(f32)
            t = np.cos(ang.astype(np.float64)) if name == "c_cosT" else np.sin(ang.astype(np.float64))
            return np.ascontiguousarray(t.T.astype(f32))
        if name == "c_ret_maskT":
            lg = np.log1p(-2.0 ** (-5.0 - np.arange(4, dtype=np.float64)))
            idx = np.arange(128)
            rel = idx[None, :] - idx[:, None]
            m = np.where(rel[None] >= 0, np.exp(lg[:, None, None] * np.maximum(rel, 0)[None]), 0.0)
            return np.ascontiguousarray(m.transpose(1, 0, 2).reshape(128, 4 * 128).astype(f32))
        if name == "c_ret_dec":
            lg = np.log1p(-2.0 ** (-5.0 - np.arange(4, dtype=np.float64)))
            idx = np.arange(128, dtype=np.float64)
            kdec = np.exp(lg[None, :] * (127.0 - idx)[:, None])
            qdec = np.exp(lg[None, :] * (idx + 1.0)[:, None])
            return np.concatenate([kdec, qdec], axis=1).astype(f32)
        if name.startswith("ml_"):
            j = int(name.split("_")[-1])
            if name.startswith("ml_w_in_"):
                return inputs["ml_w_in"][j]
            if name.startswith("ml_w_out_"):
                return inputs["ml_w_out"][j]
            if name.startswith("ml_bd_"):
                out = np.zeros((128, 48, 128), f32)
                for wi, key in enumerate(("ml_wq", "ml_wk", "ml_wv")):
                    w = inputs[key][j]
                    for cc in range(16):
                        for n_ in range(32):
                            out[4 * n_:4 * n_ + 4, wi * 16 + cc, 4 * n_:4 * n_ + 4] = w[cc * 32 + n_]
                return out.reshape(128, 48 * 128)
            if name.startswith("ml_wg_"):
                w = inputs["ml_w_gate"][j]
                return np.ascontiguousarray(w.reshape(48, 128, 8).transpose(1, 0, 2).reshape(128, 48 * 8))
            if name.startswith("ml_cw_"):
                w = inputs["ml_conv_w"][j]
                return np.ascontiguousarray(w.reshape(4, 16, 128).transpose(2, 1, 0).reshape(128, 64))
            if name.startswith("ml_cb_"):
                return np.ascontiguousarray(inputs["ml_conv_b"][j].reshape(16, 128).T)
            if name.startswith("ml_bg_bc_"):
                return bc128(np.tile(inputs["ml_b_gate"][j], S // 128))
            if name.startswith("ml_gn_bc_"):
                return bc128(inputs["ml_gn"][j])
            if name.startswith("ml_skip_bc_"):
                return bc128(inputs["ml_skip"][j])
        if name.startswith("rw_"):
            j = int(name.split("_")[-1])
            if name.startswith("rw_w_rkv_"):
                return inputs["rw_w_rkv"][j].reshape(3 * 1024, 1024)
            if name.startswith("rw_w_out_"):
                return inputs["rw_w_out"][j]
            if name.startswith("rw_lora_a_"):
                return np.concatenate([inputs["rw_w_lora_a"][j], inputs["rw_a_lora_a"][j], inputs["rw_g_lora_a"][j]], axis=1)
            if name.startswith("rw_w_lora_b_"):
                return inputs["rw_w_lora_b"][j]
            if name.startswith("rw_a_lora_b_"):
                return inputs["rw_a_lora_b"][j]
            if name.startswith("rw_g_lora_b_"):
                return inputs["rw_g_lora_b"][j]
            if name.startswith("rw_muT_"):
                m = inputs["rw_mu"][j]
                return np.ascontiguousarray(m.reshape(6, 8, 128).transpose(2, 1, 0).reshape(128, 48))
            if name.startswith("rw_vec_bc_"):
                vs = [inputs["rw_w0"][j], inputs["rw_a0"][j], inputs["rw_k_k"][j], inputs["rw_k_a"][j],
                      inputs["rw_r_k"][j].reshape(-1), inputs["rw_gn_g"][j], inputs["rw_gn_b"][j]]
                return bc128(np.concatenate(vs))
        if name == "c_rw_masks":
            i = np.arange(128)
            m0 = (i[:, None] > i[None, :]).astype(f32)
            m1 = (i[:, None] < i[None, :]).astype(f32)
            m2 = (i[:, None] <= i[None, :]).astype(f32)
            m3 = np.eye(128, dtype=f32)
            return np.concatenate([np.tile(m, (1, 4)) for m in (m0, m1, m2, m3)], axis=1)
        if name == "c_rw_mx":
            i = np.arange(128)
            tri = (i[:, None] <= i[None, :]).astype(f32)
            selmid = np.broadcast_to((i[:, None] <= 63), (128, 128)).astype(f32)
            mx1 = tri - selmid
            mx2 = 1.0 - tri
            return np.concatenate([mx1, mx2, selmid], axis=1)
        if name == "c_tri":
            return np.triu(np.ones((128, 128), f32))
        if name == "c_identf":
            return np.eye(128, dtype=f32)
        raise KeyError(name)

    def ffn(self, li):
        fw = self.fw
        nc = self.nc
        S = self.S
        P = self.P
        wgu_d = self.inp("ffn_w_gu_%d" % li, [D, 2 * DFF])
        wd_d = self.inp("ffn_w_down_%d" % li, [DFF, D])
        g_d = self.inp("norm_ffn_bc_%d" % li, [128, D])
        NJ = DFF // 128
        with contextlib.ExitStack() as st:
            wgu = fw.sb([128, 8, 2 * DFF], BF16, "wgu", st)
            wd = fw.sb([128, NJ, D], BF16, "wd", st)
            gbc = fw.sb([128, D], F32, "gbc", st)
            hb = [fw.sb([128, D], F32, "hb%d" % i, st) for i in range(4)]
            xn = [fw.sb([128, D], BF16, "xn%d" % i, st) for i in range(2)]
            sq = fw.sb([128, D], BF16, "sq", st)
            stt_ = [[fw.sb([128, 1], F32, "st", st) for _ in range(3)] for _ in range(2)]
            xnT = [fw.sb([128, 8, 512], BF16, "xnT%d" % i, st) for i in range(2)]
            sg = [fw.sb([128, 512], F32, "sg%d" % i, st) for i in range(2)]
            actT = fw.sb([128, NJ, 512], BF16, "actT", st)
            fw.dma(fw.sp, gbc[:], g_d[:, :], w=[gbc])
            self.load_w_bf16(wgu, wgu_d, D, 2 * DFF)
            self.load_w_bf16(wd, wd_d, DFF, D)
            src = self.src_tiles()
            TT = 512 if S >= 512 else S
            nsub = TT // 128
            cnt = 0
            for tt in range(S // TT):
                xT = xnT[tt % 2]
                for sub in range(nsub):
                    ti = tt * nsub + sub
                    self.norm_tile(src[ti], gbc, hb[sub], xn[cnt % 2], stt_[cnt % 2], sq)
                    self.transpose_to(xn[cnt % 2], xT, sub * 128, 8, [P[6], P[7]], [fw.act, fw.dve])
                    cnt += 1
                for j in range(NJ):
                    pg = P[(2 * j) % 4]
                    pu = P[(2 * j + 1) % 4]
                    for k in range(8):
                        self.mm(pg[:, 0:TT], wgu[:, k, j * 128:(j + 1) * 128], xT[:, k, 0:TT], k == 0, k == 7, [wgu, xT], [pg])
                    for k in range(8):
                        self.mm(pu[:, 0:TT], wgu[:, k, DFF + j * 128:DFF + (j + 1) * 128], xT[:, k, 0:TT], k == 0, k == 7, [wgu, xT], [pu])
                    s = sg[j % 2]
                    self.actf(s[:, 0:TT], pg[:, 0:TT], AF.Silu, [pg], [s])
                    self.tt(fw.dve, actT[:, j, 0:TT], s[:, 0:TT], pu[:, 0:TT], ALU.mult, [s, pu], [actT])
                for sub in range(nsub):
                    ti = tt * nsub + sub
                    hnew = hb[sub]
                    for half in range(2):
                        pb = P[4 + half]
                        for j in range(NJ):
                            self.mm(pb[:, :], actT[:, j, sub * 128:(sub + 1) * 128], wd[:, j, half * 512:(half + 1) * 512],
                                    j == 0, j == NJ - 1, [actT, wd], [pb])
                        self.tt(fw.dve, hnew[:, half * 512:(half + 1) * 512], hb[sub][:, half * 512:(half + 1) * 512], pb[:, :], ALU.add,
                                [hb[sub], pb], [hnew])
                    fw.dma(fw.sp, self.ht[ti].t, hnew[:], r=[hnew], w=[self.ht[ti]])
            fw.barrier()
        self.first = False

    def final_norm(self):
        fw = self.fw
        g_d = self.inp("norm_final_bc", [128, D])
        with contextlib.ExitStack() as st:
            gbc = fw.sb([128, D], F32, "gbc", st)
            hb = [fw.sb([128, D], F32, "hb%d" % i, st) for i in range(2)]
            ob = [fw.sb([128, D], F32, "ob%d" % i, st) for i in range(2)]
            sq = fw.sb([128, D], BF16, "sq", st)
            stt_ = [[fw.sb([128, 1], F32, "st", st) for _ in range(3)] for _ in range(2)]
            fw.dma(fw.sp, gbc[:], g_d[:, :], w=[gbc])
            src = self.src_tiles()
            for ti in range(self.NT):
                self.norm_tile(src[ti], gbc, hb[ti % 2], ob[ti % 2], stt_[ti % 2], sq)
                fw.dma(fw.sp, self.ot[ti].t, ob[ti % 2][:], r=[ob[ti % 2]], w=[self.ot[ti]])
            fw.barrier()

    def copy_out(self):
        fw = self.fw
        with contextlib.ExitStack() as st:
            hb = [fw.sb([128, D], F32, "hb%d" % i, st) for i in range(2)]
            src = self.src_tiles()
            for ti in range(self.NT):
                fw.dma(fw.sp, hb[ti % 2][:], src[ti].t, r=[src[ti]], w=[hb[ti % 2]])
                fw.dma(fw.sp, self.ot[ti].t, hb[ti % 2][:], r=[hb[ti % 2]], w=[self.ot[ti]])
            fw.barrier()


def default_layers():
    L = []
    for i in range(4):
        kind = ["ret", "mlstm", "rwkv"][i % 3]
        L.append((kind, i, i // 3))
        L.append(("ffn", i, 0))
    return L


def bc128(v):
    v = np.ascontiguousarray(v, dtype=np.float32).reshape(1, -1)
    return np.ascontiguousarray(np.broadcast_to(v, (128, v.shape[1])))


def host_inputs(prog, inputs, b):
    m = {}
    S = prog.S
    for name in prog.ins:
        if name == "x":
            m[name] = np.ascontiguousarray(inputs["x"][b])
        elif name == "c_ident":
            m[name] = np.eye(128, dtype=np.float32)
        elif name.startswith("ffn_w_gu_"):
            m[name] = np.ascontiguousarray(inputs["ffn_w_gu"][int(name.split("_")[-1])])
        elif name.startswith("ffn_w_down_"):
            m[name] = np.ascontiguousarray(inputs["ffn_w_down"][int(name.split("_")[-1])])
        elif name.startswith("norm_ffn_bc_"):
            m[name] = bc128(inputs["norm_ffn"][int(name.split("_")[-1])])
        elif name.startswith("norm_mix_bc_"):
            m[name] = bc128(inputs["norm_mix"][int(name.split("_")[-1])])
        elif name == "norm_final_bc":
            m[name] = bc128(inputs["norm_final"])
        else:
            m[name] = prog.host_extra(name, inputs)
        assert m[name].shape == prog.ins[name][0], (name, m[name].shape, prog.ins[name][0])
        m[name] = np.ascontiguousarray(m[name], dtype=np.float32)
    return m


_CACHE = {}


def run(inputs, S=None, layers=None, final_norm=True, ncores=8):
    x = np.asarray(inputs["x"])
    B = x.shape[0]
    S = S or x.shape[1]
    layers = layers if layers is not None else default_layers()
    key = (S, tuple(layers), final_norm)
    if key not in _CACHE:
        _CACHE[key] = Prog(S, layers, final_norm)
    prog = _CACHE[key]
    inputs = {k: np.asarray(v) for k, v in inputs.items()}
    maps = [host_inputs(prog, inputs, c % B) for c in range(min(B, ncores))]
    in_maps = [maps[c % B] for c in range(ncores)]
    res = run_bass_kernel_spmd(prog.nc, in_maps, core_ids=list(range(ncores)))
    LAST["res"] = res.results
    out = np.stack([np.asarray(res.results[b]["out"]) for b in range(B)], axis=0)
    return out.astype(np.float32)


def kernel(**inputs):
    return run(inputs)
```
